# Optimizing a Trainium2 kernel written in Bass

```python
import math
import jax, jax.numpy as jnp
from jax import lax
import numpy as np

D_MODEL = 1024
BATCH = 32
SEQ = 2048
DEPTH = 1

GLA_HEADS = 4
GLA_DK = 64
GLA_DV = 128
GLA_RANK = 16
GLA_CHUNK = 64
GLA_GATE_NORM = 16.0
SWA_HEADS = 8
SWA_KV_HEADS = 2
SWA_HD = 64
SWA_WINDOW = 128
SWA_BLOCK = 128
N_BRANCH = 2

GLA_QK_W = GLA_HEADS * GLA_DK
GLA_V_W = GLA_HEADS * GLA_DV
SWA_Q_W = SWA_HEADS * SWA_HD
SWA_KV_W = SWA_KV_HEADS * SWA_HD
IN_SPLITS = (GLA_QK_W, GLA_QK_W, GLA_V_W, GLA_RANK, GLA_V_W, SWA_Q_W, SWA_KV_W, SWA_KV_W, SWA_Q_W, N_BRANCH * D_MODEL)
D_IN = 4880
DEEPNORM_ALPHA = (2.0 * DEPTH) ** 0.25
DEEPNORM_BETA = (8.0 * DEPTH) ** -0.25
LN_EPS = 1e-5
RMS_EPS = 1e-6

kernel_name = "hybrid_gla_swa_sink_alibi_deepnorm"


def _alibi_slopes(n):
    return 2.0 ** (-8.0 * jnp.arange(1, n + 1, dtype=jnp.float32) / n)


def _gla(q, k, v, log_g):
    B, S, H, DK = q.shape
    DV = v.shape[-1]
    L = GLA_CHUNK
    C = S // L
    q = q.reshape(B, C, L, H, DK) * (DK ** -0.5)
    k = k.reshape(B, C, L, H, DK)
    v = v.reshape(B, C, L, H, DV)
    G = lax.cumsum(log_g.reshape(B, C, L, H, DK), axis=2)
    G_last = G[:, :, -1]
    q_dec = q * jnp.exp(G)
    k_inv = k * jnp.exp(-G)
    causal = jnp.tril(jnp.ones((L, L), dtype=bool))
    A = jnp.einsum('bclhd,bcmhd->bchlm', q_dec, k_inv)
    A = jnp.where(causal, A, 0.0)
    o_intra = jnp.einsum('bchlm,bcmhv->bclhv', A, v)
    k_to_end = k * jnp.exp(G_last[:, :, None] - G)
    kv = jnp.einsum('bclhd,bclhv->cbhdv', k_to_end, v)
    decay = jnp.exp(G_last).transpose(1, 0, 2, 3)

    def step(state, inp):
        d, kv_c = inp
        return state * d[..., None] + kv_c, state

    s0 = jnp.zeros((B, H, DK, DV), dtype=q.dtype)
    _, s_prev = lax.scan(step, s0, (decay, kv))
    o_inter = jnp.einsum('bclhd,cbhdv->bclhv', q_dec, s_prev)
    return (o_intra + o_inter).reshape(B, S, H, DV)


def _swa_sink_alibi(q, k, v, sinks):
    B, S, H, D = q.shape
    KV = k.shape[2]
    G = H // KV
    W = SWA_BLOCK
    N = S // W
    qb = q.reshape(B, N, W, KV, G, D)
    pad = ((0, 0), (W, 0), (0, 0), (0, 0))
    kp = jnp.pad(k, pad).reshape(B, N + 1, W, KV, D)
    vp = jnp.pad(v, pad).reshape(B, N + 1, W, KV, D)
    kb = jnp.concatenate([kp[:, :-1], kp[:, 1:]], axis=2)
    vb = jnp.concatenate([vp[:, :-1], vp[:, 1:]], axis=2)
    scores = jnp.einsum('bnqkgd,bnskd->bnkgqs', qb, kb).astype(jnp.float32) * (D ** -0.5)
    q_pos = jnp.arange(N)[:, None] * W + jnp.arange(W)[None, :]
    k_pos = (jnp.arange(N)[:, None] - 1) * W + jnp.arange(2 * W)[None, :]
    dist = q_pos[:, :, None] - k_pos[:, None, :]
    valid = (dist >= 0) & (dist < SWA_WINDOW) & (k_pos[:, None, :] >= 0)
    slopes = _alibi_slopes(H).reshape(KV, G)
    scores = scores - slopes[None, None, :, :, None, None] * dist.astype(jnp.float32)[None, :, None, None]
    scores = jnp.where(valid[None, :, None, None], scores, -jnp.inf)
    sink = jnp.broadcast_to(sinks.astype(jnp.float32).reshape(1, 1, KV, G, 1, 1), scores.shape[:-1] + (1,))
    probs = jax.nn.softmax(jnp.concatenate([scores, sink], axis=-1), axis=-1)[..., :-1]
    out = jnp.einsum('bnkgqs,bnskd->bnqkgd', probs.astype(v.dtype), vb)
    return out.reshape(B, S, H * D)


def _mixer(x, w_in, w_gk2, b_gk, gla_norm_g, w_o_gla, sinks, w_o_swa, b_gate, w_out):
    B, S, _ = x.shape
    h = x @ w_in
    idx = tuple(int(i) for i in np.cumsum(IN_SPLITS)[:-1])
    q_g, k_g, v_g, gk_lr, z_g, q_s, k_s, v_s, z_s, gate_logits = jnp.split(h, idx, axis=-1)

    log_g = jax.nn.log_sigmoid((gk_lr @ w_gk2 + b_gk).astype(jnp.float32)) / GLA_GATE_NORM
    o = _gla(q_g.astype(jnp.float32).reshape(B, S, GLA_HEADS, GLA_DK),
             k_g.astype(jnp.float32).reshape(B, S, GLA_HEADS, GLA_DK),
             v_g.astype(jnp.float32).reshape(B, S, GLA_HEADS, GLA_DV),
             log_g.reshape(B, S, GLA_HEADS, GLA_DK))
    o = o * lax.rsqrt(jnp.mean(o * o, axis=-1, keepdims=True) + RMS_EPS) * gla_norm_g.astype(jnp.float32)
    y_gla = (o.reshape(B, S, GLA_V_W).astype(x.dtype) * jax.nn.silu(z_g)) @ w_o_gla

    o_s = _swa_sink_alibi(q_s.reshape(B, S, SWA_HEADS, SWA_HD),
                          k_s.reshape(B, S, SWA_KV_HEADS, SWA_HD),
                          v_s.reshape(B, S, SWA_KV_HEADS, SWA_HD), sinks)
    y_swa = (o_s * jax.nn.silu(z_s)) @ w_o_swa

    gates = jax.nn.sigmoid(gate_logits + b_gate)
    g_a, g_b = jnp.split(gates, 2, axis=-1)
    return (g_a * y_gla + g_b * y_swa) @ w_out


def setup_inputs(seed: int = 0) -> dict:
    key = jax.random.key(seed)
    ks = jax.random.split(key, 12)
    beta = DEEPNORM_BETA
    col_scales = (1.0, 1.0, beta, 1.0, 1.0, 1.0, 1.0, beta, 1.0, 1.0)
    col_scale = jnp.concatenate([jnp.full((n,), s, dtype=jnp.float32) for n, s in zip(IN_SPLITS, col_scales)])
    x = jax.random.normal(ks[0], (BATCH, SEQ, D_MODEL), jnp.float32)
    w_in = jax.random.normal(ks[1], (D_MODEL, D_IN), jnp.float32) * (D_MODEL ** -0.5) * col_scale
    w_gk2 = jax.random.normal(ks[2], (GLA_RANK, GLA_QK_W), jnp.float32) * (GLA_RANK ** -0.5)
    b_gk = 0.01 * jax.random.normal(ks[3], (GLA_QK_W,), jnp.float32)
    gla_norm_g = 1.0 + 0.02 * jax.random.normal(ks[4], (GLA_DV,), jnp.float32)
    w_o_gla = jax.random.normal(ks[5], (GLA_V_W, D_MODEL), jnp.float32) * (GLA_V_W ** -0.5) * beta
    sinks = 0.5 * jax.random.normal(ks[6], (SWA_HEADS,), jnp.float32)
    w_o_swa = jax.random.normal(ks[7], (SWA_Q_W, D_MODEL), jnp.float32) * (SWA_Q_W ** -0.5) * beta
    b_gate = 0.01 * jax.random.normal(ks[8], (N_BRANCH * D_MODEL,), jnp.float32)
    w_out = jax.random.normal(ks[9], (D_MODEL, D_MODEL), jnp.float32) * (D_MODEL ** -0.5) * beta
    ln_g = 1.0 + 0.02 * jax.random.normal(ks[10], (D_MODEL,), jnp.float32)
    ln_b = 0.02 * jax.random.normal(ks[11], (D_MODEL,), jnp.float32)
    return {"x": x, "w_in": w_in, "w_gk2": w_gk2, "b_gk": b_gk, "gla_norm_g": gla_norm_g,
            "w_o_gla": w_o_gla, "sinks": sinks, "w_o_swa": w_o_swa, "b_gate": b_gate,
            "w_out": w_out, "ln_g": ln_g, "ln_b": ln_b}


def reference(x, w_in, w_gk2, b_gk, gla_norm_g, w_o_gla, sinks, w_o_swa, b_gate, w_out, ln_g, ln_b):
    for _ in range(DEPTH):
        y = _mixer(x, w_in, w_gk2, b_gk, gla_norm_g, w_o_gla, sinks, w_o_swa, b_gate, w_out)
        r = (DEEPNORM_ALPHA * x + y).astype(jnp.float32)
        mu = jnp.mean(r, axis=-1, keepdims=True)
        var = jnp.mean(jnp.square(r - mu), axis=-1, keepdims=True)
        x = ((r - mu) * lax.rsqrt(var + LN_EPS) * ln_g.astype(jnp.float32) + ln_b.astype(jnp.float32)).astype(x.dtype)
    return x
```

```python
import os
from contextlib import ExitStack

import numpy as np
import concourse.bass as bass
import concourse.mybir as mybir
from concourse.bass_utils import run_bass_kernel_spmd

F32 = mybir.dt.float32
BF16 = mybir.dt.bfloat16
AF = mybir.ActivationFunctionType
ALU = mybir.AluOpType

N_CORES = 8
D = 1024
SEQ = 2048
SEQ_PER_CORE = 4
NTOK = SEQ * SEQ_PER_CORE
T = 512
NSB_SEQ = SEQ // T
D_IN = 4880
ALPHA = 2.0 ** 0.25
LN_EPS = 1e-5
RMS_EPS = 1e-6

C_QG, C_KG, C_VG, C_GK, C_ZG, C_QS, C_KS, C_VS, C_ZS, C_GATE = (
    0, 256, 512, 1024, 1040, 1552, 2064, 2192, 2320, 2832)
W_GROUPS = [(0, 512), (512, 1040), (1040, 1552), (1552, 2064), (2064, 2320),
            (2320, 2832), (2832, 3344), (3344, 3856), (3856, 4368), (4368, 4880)]


def pskeys(bank):
    return ["ps%d" % bank]


def wgrp(c):
    for i, (a, b) in enumerate(W_GROUPS):
        if a <= c < b:
            return "w_in.%d" % i
    raise ValueError(c)


class _Op:
    __slots__ = ("eng", "fn", "raw", "oth", "dma", "need_inc", "done", "waits")

    def __init__(self, eng, fn, raw, oth, dma):
        self.eng, self.fn, self.raw, self.oth, self.dma = eng, fn, raw, oth, dma
        self.need_inc = False
        self.done = None
        self.waits = []


class Sched:
    ENGS = ("pe", "act", "dve", "pool", "sp")

    def __init__(self):
        self.ops = []
        self.last_w = {}
        self.readers = {}

    def add(self, eng, fn, reads=(), writes=(), dma=None):
        i = len(self.ops)
        raw, oth = set(), set()
        for r in reads:
            w = self.last_w.get(r)
            if w is not None:
                raw.add(w)
        for k in writes:
            w = self.last_w.get(k)
            if w is not None:
                raw.add(w)
            for rd in self.readers.get(k, ()):
                oth.add(rd)
        for r in reads:
            self.readers.setdefault(r, []).append(i)
        for k in writes:
            self.last_w[k] = i
            self.readers[k] = []
        self.ops.append(_Op(eng, fn, raw, oth - raw, dma))
        return i

    def finalize(self):
        ops = self.ops
        for op in ops:
            deps = set()
            for d in op.raw:
                p = ops[d]
                deps.add(d)
            for d in op.oth:
                p = ops[d]
                if p.eng == op.eng and p.dma is None:
                    continue
                deps.add(d)
            if op.eng == "pe":
                deps = {d for d in deps if not (ops[d].eng == "pe" and ops[d].dma is None)}
            op.raw = deps
            for d in deps:
                ops[d].need_inc = True
        cnt = {}
        self.sem_keys = []
        for op in ops:
            if op.dma is not None:
                key = ("dma", op.dma)
                cnt[key] = cnt.get(key, 0) + 16
                op.done = (key, cnt[key])
            elif op.need_inc:
                key = ("eng", op.eng)
                cnt[key] = cnt.get(key, 0) + 1
                op.done = (key, cnt[key])
            else:
                continue
            if key not in self.sem_keys:
                self.sem_keys.append(key)
        self.final_cnt = cnt
        seen = {e: {} for e in self.ENGS}
        for op in ops:
            w = {}
            for d in op.raw:
                key, c = ops[d].done
                if w.get(key, 0) < c:
                    w[key] = c
            sw = seen[op.eng]
            op.waits = []
            for key, c in w.items():
                if sw.get(key, 0) < c:
                    op.waits.append((key, c))
                    sw[key] = c

    def emit(self, nc, block, sems, final_waits):
        reg = {"pe": block.tensor, "act": block.scalar, "dve": block.vector,
               "pool": block.gpsimd, "sp": block.sync}
        for eng in self.ENGS:
            ops_e = [op for op in self.ops if op.eng == eng]

            def body(e, ops_e=ops_e, eng=eng):
                for op in ops_e:
                    for key, c in op.waits:
                        e.wait_ge(sems[key], c)
                    ins = op.fn(e)
                    if op.dma is not None:
                        ins.then_inc(sems[("dma", op.dma)], 16)
                    elif op.need_inc:
                        ins.then_inc(sems[("eng", eng)], 1)
                if eng == "sp":
                    for key in final_waits:
                        e.wait_ge(sems[key], self.final_cnt[key])

            reg[eng](body)


def _interleave(gens):
    gens = list(gens)
    while gens:
        for g in list(gens):
            try:
                next(g)
            except StopIteration:
                gens.remove(g)


def build_nc(sb_list):
    nc = bass.Bass("TRN2", target_bir_lowering=False)

    def din(name, shape, dt=F32):
        return nc.dram_tensor(name, list(shape), dt, kind="ExternalInput").ap()

    x_d = din("x", [NTOK, D])
    w_in_d = din("w_in", [D, D_IN])
    w_og_d = din("w_o_gla", [512, D])
    w_os_d = din("w_o_swa", [512, D])
    w_out_d = din("w_out", [D, D])
    wgk_d = din("wgk_aug", [32, 256])
    gnorm_d = din("gnorm", [128, 1])
    sinks_d = din("sinks_rep", [2, 512])
    bgate_d = din("bgate", [128, 16])
    lng_d = din("lng_bc", [128, D])
    lnb_d = din("lnb_bc", [128, D])
    c_ident_d = din("c_ident", [128, 128])
    c_mask_d = din("c_mask", [128, 512])
    c_mb_d = din("c_mb", [128, 2048])
    c_E_d = din("c_E", [128, 256])
    c_sel_d = din("c_sel", [2, 128])
    c_gk_d = din("c_gk", [16, 512])
    out_d = nc.dram_tensor("out", [NTOK, D], F32, kind="ExternalOutput").ap()

    S = Sched()
    es = ExitStack()

    def sb(name, shape, dt):
        return es.enter_context(nc.sbuf_tensor(name, list(shape), dt))

    with es:
        w_in_sb = sb("w_in_sb", [128, 8, D_IN], BF16)
        w_og_sb = sb("w_og_sb", [128, 4, D], BF16)
        w_os_sb = sb("w_os_sb", [128, 4, D], BF16)
        w_out_sb = sb("w_out_sb", [128, 8, D], BF16)
        ident = sb("ident", [128, 128], BF16)
        tri = sb("tri", [128, 128], F32)
        mb = sb("mb", [128, 4, 512], BF16)
        ones = sb("ones", [128, 128], BF16)
        Emat = sb("Emat", [128, 2, 128], BF16)
        wgk_t = sb("wgk", [36, 256], BF16)
        wgk = wgk_t[0:32, :]
        selT = wgk_t
        gnorm = sb("gnorm_sb", [128, 1], F32)
        bgate = sb("bgate_sb", [128, 16], F32)
        lng = sb("lng", [128, D], F32)
        lnb = sb("lnb", [128, D], F32)

        xbf = [sb("xbf%d" % i, [128, D], BF16) for i in range(4)]
        xT = sb("xT", [128, 8, T], BF16)
        qgT = sb("qgT", [128, 2, T], BF16)
        kgT = sb("kgT", [128, 2, T], BF16)
        szg = sb("szg", [128, 4, T], BF16)
        qsT = sb("qsT", [128, 4, T], BF16)
        ksz = sb("ksz", [128, 2, T], BF16)
        szs = sb("szs", [128, 4, T], BF16)
        smallT = sb("smallT", [128, T], BF16)
        gkT = smallT[0:32, :]
        sink_hi = smallT[32:34, :]
        sink_lo = smallT[34:36, :]
        sel_hi = selT[32:34, 0:128]
        sel_lo = selT[34:36, 0:128]
        vg_tok = sb("vg_tok", [128, 4, 512], BF16)
        vs_pad = sb("vs_pad", [128, 4, 256], BF16)
        ks_carry = sb("ks_carry", [128, 2, 128], BF16)
        vs_carry = sb("vs_carry", [128, 256], BF16)

        sp_sb = sb("sp_sb", [128, 256], F32)
        eGi = sb("eGi", [128, 2, 128], F32)
        dec = [sb("dec%d" % i, [128, 2, 1], F32) for i in range(4)]
        decm = sb("decm", [128, 2, 2, 1], F32)
        halfmask = sb("halfmask", [128, 2, 2, 1], F32)
        mhalf = sb("mhalf", [128, 1], F32)
        vpe = sb("vpe", [128, 1], F32)
        q_dec = sb("q_dec", [128, 2, 128], BF16)
        kinv_tok = sb("kinv_tok", [128, 256], BF16)
        k_inv = kinv_tok[:].rearrange("p (c t) -> p c t", c=2)
        AT_m = sb("AT_m", [128, 512], BF16)
        Rst = sb("Rst", [128, 2, 128], F32)
        Sz = sb("Sz", [128, 2, 2, 128], BF16)
        kz = sb("kz", [128, 2, 2, 128], BF16)
        rs = sb("rs", [128, 512], F32)
        u_sb = sb("u_sb", [128, 4, 128], BF16)
        p_sb = sb("p_sb", [128, 4, 512], BF16)
        rden = sb("rden", [128, 512], F32)
        t_sb = sb("t_sb", [128, 4, 128], BF16)
        og_gT = sb("og_gT", [128, 4, T], BF16)
        os_gT = sb("os_gT", [128, 4, T], BF16)

        ga_sb = sb("ga_sb", [128, T], BF16)
        gb_sb = sb("gb_sb", [128, T], BF16)
        mT = sb("mT", [128, 8, T], BF16)

        xr = [sb("xr%d" % i, [128, D], F32) for i in range(2)]
        st12 = sb("st12", [128, 2], F32)
        ms12 = sb("ms12", [128, 2], F32)
        nvar = sb("nvar", [128, 1], F32)
        rstd = sb("rstd", [128, 1], F32)
        nbias = sb("nbias", [128, 1], F32)

        eG = sp_sb[:].rearrange("p (c t) -> p c t", c=2)
        t1 = u_sb[:].rearrange("p h t -> p (h t)")
        t2 = t_sb[:].rearrange("p h t -> p (h t)")
        PS = [es.enter_context(nc.psum_tensor("ps%d" % i, [128, 512], F32)) for i in range(8)]
        PSb7 = PS[7].bitcast(BF16)
        PSb0 = PS[0].bitcast(BF16)
        junk = rs.bitcast(BF16)

        def dma(eng, key, out, in_, reads=(), writes=()):
            S.add(eng, lambda e, o=out, i=in_: e.dma_start(out=o, in_=i),
                  reads=reads, writes=writes, dma=key)

        w_in_v = w_in_d.rearrange("(kc p) n -> p kc n", p=128)
        dma("pool", "c_ident", ident[:], c_ident_d, writes=["ident"])
        dma("pool", "c_gk", smallT[16:32, :], c_gk_d, writes=["gkT.c"])
        def wload(gi):
            a, b = W_GROUPS[gi]
            dma("pool", "w_in.%d" % gi, w_in_sb[:, :, a:b], w_in_v[:, :, a:b], writes=["w_in.%d" % gi])

        first_s = sb_list[0]
        for b in range(4):
            gb0 = first_s * 4 + b
            dma("pool", "xbf%d" % b, xbf[b][:], x_d[gb0 * 128:(gb0 + 1) * 128, :], writes=["xbf%d" % b])
        wload(0)
        wload(1)
        dma("pool", "c_wgk", wgk, wgk_d, writes=["wgk"])
        dma("sp", "c_tri", tri[:], c_mask_d[:, 0:128], writes=["tri"])
        dma("sp", "c_gnorm", gnorm[:], gnorm_d, writes=["gnorm"])
        sink_f = rden[0:2, :]
        sink_hi32 = rs[0:2, :]
        dma("sp", "c_sinks", sink_f, sinks_d, writes=["rden"])
        dma("sp", "c_bgate", bgate[:], bgate_d, writes=["bgate"])
        dma("sp", "c_lng", lng[:], lng_d, writes=["lng"])
        dma("sp", "c_lnb", lnb[:], lnb_d, writes=["lnb"])
        for gi in (3, 4, 2, 5):
            wload(gi)
        dma("pool", "c_mb", mb[:].rearrange("p a b -> p (a b)"), c_mb_d, writes=["mb"])
        dma("pool", "c_E", Emat[:].rearrange("p a b -> p (a b)"), c_E_d, writes=["Emat"])
        dma("pool", "c_sel", sel_hi, c_sel_d, writes=["sel"])
        dma("pool", "c_sel2", sel_lo, c_sel_d, writes=["sel2"])
        for gi in (6, 8):
            wload(gi)
        dma("pool", "w_os", w_os_sb[:], w_os_d.rearrange("(kc p) n -> p kc n", p=128), writes=["w_os"])
        dma("pool", "w_og", w_og_sb[:], w_og_d.rearrange("(kc p) n -> p kc n", p=128), writes=["w_og"])
        for gi in (7, 9):
            wload(gi)
        dma("pool", "w_out", w_out_sb[:], w_out_d.rearrange("(kc p) n -> p kc n", p=128), writes=["w_out"])

        S.add("dve", lambda e: e.memset(ones[:], 1.0), writes=["ones"])
        S.add("dve", lambda e: e.memset(vs_pad[:], 0.0), writes=["vs_pad.%d" % b for b in range(4)])
        S.add("dve", lambda e: e.memset(vs_carry[:], 0.0), writes=["vs_carry"])
        S.add("dve", lambda e: e.memset(ksz[:], 0.0), writes=["ksT"])
        S.add("dve", lambda e: e.memset(ks_carry[:], 0.0), writes=["ks_carry"])
        S.add("dve", lambda e: e.memset(Sz[:], 0.0), writes=["Sbf"])
        S.add("dve", lambda e: e.memset(kz[:], 0.0), writes=["kz"])
        S.add("dve", lambda e: e.memset(mhalf[:], -0.5), writes=["mhalf"])
        S.add("dve", lambda e: e.memset(halfmask[:], 0.0), writes=["halfmask"])
        S.add("dve", lambda e: e.memset(halfmask[0:64, :, 0, :], 1.0), writes=["halfmask"])
        S.add("dve", lambda e: e.memset(halfmask[64:128, :, 1, :], 1.0), writes=["halfmask"])
        S.add("act", lambda e: e.activation(out=sink_f, in_=sink_f, func=AF.Exp),
              reads=["rden"], writes=["rden"])
        sink_t = p_sb[0:2, 0:2, :]
        S.add("dve", lambda e: e.tensor_copy(out=sink_t[:, 0, :], in_=sink_f), reads=["rden"], writes=["p.0"])
        S.add("dve", lambda e: e.tensor_copy(out=sink_hi32, in_=sink_t[:, 0, :]), reads=["p.0"], writes=["rs"])
        S.add("dve", lambda e: e.tensor_tensor(out=sink_t[:, 1, :], in0=sink_f, in1=sink_hi32, op=ALU.subtract),
              reads=["rden", "rs"], writes=["p.1"])
        dma("sp", "c_sh", sink_hi, sink_t[:, 0, :], reads=["p.0"], writes=["sink_hi"])
        dma("sp", "c_sl", sink_lo, sink_t[:, 1, :], reads=["p.1"], writes=["sink_lo"])

        W_ALL = ["w_in.%d" % i for i in range(10)]
        XT_ALL = ["xT.%d" % b for b in range(4)]

        def emit_xload(s, b):
            gb = s * 4 + b
            slot = b
            dma("pool", "xbf%d" % slot, xbf[slot][:], x_d[gb * 128:(gb + 1) * 128, :],
                writes=["xbf%d" % slot])

        def phase0(s, preloaded, nxt=None):
            for b in range(4):
                if b not in preloaded:
                    emit_xload(s, b)
                slot = b
                pbk, pkey = ((PSb7, "ps7"), (PSb0, "ps0"))[b % 2]
                for kc in range(8):
                    S.add("pe", lambda e, kc=kc, slot=slot, pbk=pbk: e.transpose(
                        out=pbk[:, kc * 128:(kc + 1) * 128], in_=xbf[slot][:, kc * 128:(kc + 1) * 128],
                        identity=ident[:]),
                        reads=["xbf%d" % slot, "ident"], writes=[pkey])
                S.add(("dve", "act")[b % 2], lambda e, b=b, pbk=pbk: (
                    e.tensor_copy(out=xT[:, :, b * 128:(b + 1) * 128],
                                  in_=pbk[:].rearrange("p (k t) -> p k t", k=8)) if b % 2 == 0 else
                    e.activation(out=xT[:, :, b * 128:(b + 1) * 128],
                                 in_=pbk[:].rearrange("p (k t) -> p k t", k=8), func=AF.Copy)),
                    reads=[pkey], writes=["xT.%d" % b])
                if nxt is not None:
                    emit_xload(nxt, b)

        def phase1(s):
            chunks = []
            for c in range(2):
                chunks.append(("qg", c, C_QG + c * 128, 128))
            for c in range(2):
                chunks.append(("kg", c, C_KG + c * 128, 128))
            chunks.append(("gk", 0, C_GK, 16))
            for c in range(4):
                chunks.append(("qs", c, C_QS + c * 128, 128))
                chunks.append(("zg", c, C_ZG + c * 128, 128))
            chunks.append(("ks", 0, C_KS, 128))
            for c in range(4):
                chunks.append(("zs", c, C_ZS + c * 128, 128))
            for i, (kind, c, c0, M) in enumerate(chunks):
                if i > 0:
                    yield
                bank = (1, 6, 2, 3)[i % 4] if i < 14 else (1, 6)[i % 2]
                for kc in range(8):
                    if kc == 4:
                        yield
                    S.add("pe", lambda e, kc=kc, bank=bank, c0=c0, M=M: e.matmul(
                        PS[bank][0:M, :], lhsT=w_in_sb[:, kc, c0:c0 + M], rhs=xT[:, kc, :],
                        start=(kc == 0), stop=(kc == 7)),
                        reads=[wgrp(c0)] + XT_ALL, writes=pskeys(bank))
                src = PS[bank]
                rd = pskeys(bank)
                if kind == "qg":
                    S.add("dve", lambda e, c=c, src=src: e.tensor_scalar(
                        out=qgT[:, c, :], in0=src[:], scalar1=0.125, scalar2=None, op0=ALU.mult),
                        reads=rd, writes=["qgT"])
                elif kind == "kg":
                    S.add("dve", lambda e, c=c, src=src: e.tensor_copy(out=kgT[:, c, :], in_=src[:]),
                          reads=rd, writes=["kgT"])
                elif kind == "gk":
                    S.add("dve", lambda e, src=src: e.tensor_copy(out=smallT[0:16, :], in_=src[0:16, :]),
                          reads=rd, writes=["gkT"])
                elif kind == "qs":
                    S.add("dve", lambda e, c=c, src=src: e.tensor_scalar(
                        out=qsT[:, c, :], in0=src[:], scalar1=0.125, scalar2=None, op0=ALU.mult),
                        reads=rd, writes=["qsT"])
                elif kind == "ks":
                    for kv in range(2):
                        S.add("dve", lambda e, src=src, kv=kv: e.tensor_copy(
                            out=ksz[kv * 64:(kv + 1) * 64, kv, :], in_=src[kv * 64:(kv + 1) * 64, :]),
                            reads=rd, writes=["ksT"])
                elif kind == "zg":
                    S.add("act", lambda e, c=c, src=src: e.activation(
                        out=szg[:, c, :], in_=src[:], func=AF.Silu),
                        reads=rd, writes=["szg"])
                elif kind == "zs":
                    S.add("act", lambda e, c=c, src=src: e.activation(
                        out=szs[:, c, :], in_=src[:], func=AF.Silu),
                        reads=rd, writes=["szs"])
            for b in range(4):
                yield
                for kc in range(8):
                    S.add("pe", lambda e, kc=kc, b=b: e.matmul(
                        PS[2][:, :], lhsT=xT[:, kc, b * 128:(b + 1) * 128], rhs=w_in_sb[:, kc, C_VG:C_VG + 512],
                        start=(kc == 0), stop=(kc == 7)),
                        reads=[wgrp(C_VG), "xT.%d" % b], writes=["ps2"])
                for kc in range(8):
                    S.add("pe", lambda e, kc=kc, b=b: e.matmul(
                        PS[3][:, 0:128], lhsT=xT[:, kc, b * 128:(b + 1) * 128], rhs=w_in_sb[:, kc, C_VS:C_VS + 128],
                        start=(kc == 0), stop=(kc == 7)),
                        reads=[wgrp(C_VS), "xT.%d" % b], writes=["ps3"])
                S.add("dve", lambda e, b=b: e.tensor_copy(out=vg_tok[:, b, :], in_=PS[2][:, :]),
                      reads=["ps2"], writes=["vg_tok.%d" % b])
                for kv in range(2):
                    S.add("act", lambda e, b=b, kv=kv: e.activation(
                        out=vs_pad[:, b, kv * 128 + kv * 64: kv * 128 + kv * 64 + 64],
                        in_=PS[3][:, kv * 64:(kv + 1) * 64], func=AF.Copy),
                        reads=["ps3"], writes=["vs_pad.%d" % b])

        def gla(s, b):
            first = (s % NSB_SEQ == 0 and b == 0)
            gb = s * 4 + b
            dcur, dprev = dec[gb % 4], dec[(gb + 3) % 4]
            dck, dpk = "dec%d" % (gb % 4), "dec%d" % ((gb + 3) % 4)
            bs = slice(b * 128, (b + 1) * 128)
            PA, pak = (PS[1], "ps1") if b % 2 == 0 else (PS[7], "ps7")
            S.add("pe", lambda e: e.matmul(PS[0][:, 0:256], lhsT=smallT[0:32, bs], rhs=wgk, start=True, stop=True),
                  reads=["gkT", "gkT.c", "wgk"], writes=["ps0"])
            yield
            S.add("act", lambda e: e.activation(out=sp_sb[:], in_=PS[0][:, 0:256], func=AF.Exp, scale=-1.0),
                  reads=["ps0"], writes=["sp_sb"])
            yield
            S.add("act", lambda e: e.activation(out=sp_sb[:], in_=sp_sb[:], func=AF.Ln, bias=1.0),
                  reads=["sp_sb"], writes=["sp_sb"])
            yield
            for c in range(2):
                S.add("pe", lambda e, c=c: e.matmul(
                    PS[0][:, 256 + c * 128:256 + (c + 1) * 128], lhsT=sp_sb[:, c * 128:(c + 1) * 128], rhs=tri[:],
                    start=True, stop=True),
                    reads=["sp_sb", "tri"], writes=["ps0"])
            yield
            GTv = PS[0][:, 256:512].rearrange("p (c t) -> p c t", c=2)
            S.add("act", lambda e: e.activation(out=eG, in_=GTv, func=AF.Exp, scale=-1.0 / 16.0),
                  reads=["ps0"], writes=["sp_sb"])
            S.add("act", lambda e: e.activation(out=eGi[:], in_=GTv, func=AF.Exp, scale=1.0 / 16.0),
                  reads=["ps0"], writes=["eGi"])
            S.add("act", lambda e: e.activation(out=dcur[:], in_=GTv[:, :, 127:128], func=AF.Exp, scale=-1.0 / 16.0),
                  reads=["ps0"], writes=[dck])
            yield
            S.add("dve", lambda e: e.tensor_tensor(out=k_inv, in0=kgT[:, :, bs], in1=eGi[:], op=ALU.mult),
                  reads=["kgT", "eGi"], writes=["kinv_tok"])
            S.add("dve", lambda e: e.tensor_tensor(out=q_dec[:], in0=qgT[:, :, bs], in1=eG, op=ALU.mult),
                  reads=["qgT", "sp_sb"], writes=["q_dec"])
            for hh in range(2):
                hs = slice(hh * 64, hh * 64 + 64)
                S.add("dve", lambda e, hh=hh, hs=hs: e.tensor_tensor(
                    out=kz[hs, :, hh, :], in0=kgT[hs, :, bs], in1=eGi[hs, :, :], op=ALU.mult),
                    reads=["kgT", "eGi"], writes=["kz"])
            yield
            for c in range(2):
                S.add("pe", lambda e, c=c: e.transpose(
                    out=PSb0[:, c * 128:(c + 1) * 128], in_=k_inv[:, c, :], identity=ident[:]),
                    reads=["kinv_tok", "ident"], writes=["ps0"])
            yield
            S.add("dve", lambda e: e.tensor_copy(out=kinv_tok[:], in_=PSb0[:, 0:256]),
                  reads=["ps0"], writes=["kinv_tok"])
            yield
            for h in range(4):
                c, hs = h // 2, slice((h % 2) * 64, (h % 2) * 64 + 64)
                S.add("pe", lambda e, h=h, c=c: e.matmul(
                    PA[:, h * 128:(h + 1) * 128], lhsT=kz[:, c, h % 2, :], rhs=q_dec[:, c, :],
                    start=True, stop=True),
                    reads=["kz", "q_dec"], writes=[pak])
            yield
            S.add("dve", lambda e: e.tensor_tensor(
                out=AT_m[:].rearrange("p (h t) -> p h t", h=4), in0=PA[:, :].rearrange("p (h t) -> p h t", h=4),
                in1=tri[:].unsqueeze(1).to_broadcast([128, 4, 128]), op=ALU.mult),
                reads=[pak, "tri"], writes=["AT_m"])
            yield
            for h in range(4):
                c, hs = h // 2, slice((h % 2) * 64, (h % 2) * 64 + 64)
                S.add("pe", lambda e, h=h: e.matmul(
                    PA[:, h * 128:(h + 1) * 128], lhsT=vg_tok[:, b, h * 128:(h + 1) * 128],
                    rhs=AT_m[:, h * 128:(h + 1) * 128], start=True, stop=first),
                    reads=["vg_tok.%d" % b, "AT_m"], writes=[pak])
                if not first:
                    S.add("pe", lambda e, h=h, c=c: e.matmul(
                        PA[:, h * 128:(h + 1) * 128], lhsT=Sz[:, c, h % 2, :], rhs=q_dec[:, c, :],
                        start=False, stop=True),
                        reads=["Sbf", "q_dec"], writes=[pak])
            yield
            for c in range(2):
                S.add("pe", lambda e, c=c: e.matmul(
                    PS[6][:, c * 256:(c + 1) * 256], lhsT=kinv_tok[:, c * 128:(c + 1) * 128],
                    rhs=vg_tok[:, b, c * 256:(c + 1) * 256], start=True, stop=True),
                    reads=["kinv_tok", "vg_tok.%d" % b], writes=["ps6"])
            yield
            for c in range(2):
                for hh in range(2):
                    hs = slice(hh * 64, hh * 64 + 64)
                    src = PS[6][hs, c * 256 + hh * 128: c * 256 + hh * 128 + 128]
                    if first:
                        S.add("dve", lambda e, c=c, hs=hs, src=src: e.tensor_copy(out=Rst[hs, c, :], in_=src),
                              reads=["ps6"], writes=["Rst"])
                    else:
                        S.add("dve", lambda e, c=c, hs=hs, src=src: e.scalar_tensor_tensor(
                            out=Rst[hs, c, :], in0=Rst[hs, c, :], scalar=dprev[hs, c, :], in1=src,
                            op0=ALU.mult, op1=ALU.add),
                            reads=["ps6", "Rst", dpk], writes=["Rst"])
            yield
            S.add("dve", lambda e: e.tensor_tensor(
                out=decm[:], in0=dcur[:].unsqueeze(2).to_broadcast([128, 2, 2, 1]), in1=halfmask[:], op=ALU.mult),
                reads=[dck, "halfmask"], writes=["decm"])
            S.add("dve", lambda e: e.tensor_tensor(
                out=Sz[:], in0=Rst[:].unsqueeze(2).to_broadcast([128, 2, 2, 128]),
                in1=decm[:].to_broadcast([128, 2, 2, 128]), op=ALU.mult),
                reads=["Rst", "decm"], writes=["Sbf"])
            yield
            S.add("act", lambda e: e.activation(out=AT_m[:], in_=PA[:, :], func=AF.Square),
                  reads=[pak], writes=["AT_m"])
            yield
            S.add("pe", lambda e: e.matmul(PS[6][:, :], lhsT=ones[:], rhs=AT_m[:], start=True, stop=True),
                  reads=["ones", "AT_m"], writes=["ps6"])
            yield
            S.add("act", lambda e: e.activation(out=rs[:], in_=PS[6][:, :], func=AF.Ln, scale=1.0 / 128.0, bias=RMS_EPS),
                  reads=["ps6"], writes=["rs"])
            yield
            S.add("act", lambda e: e.activation(out=rs[:], in_=rs[:], func=AF.Exp, scale=-0.5),
                  reads=["rs"], writes=["rs"])
            yield
            S.add("dve", lambda e: e.scalar_tensor_tensor(
                out=u_sb[:], in0=rs[:].rearrange("p (h t) -> p h t", h=4), scalar=gnorm[:, 0:1],
                in1=szg[:, :, bs], op0=ALU.mult, op1=ALU.mult),
                reads=["rs", "gnorm", "szg"], writes=["u_sb"])
            yield
            S.add("dve", lambda e: e.tensor_tensor(
                out=og_gT[:, :, bs], in0=PA[:, :].rearrange("p (h t) -> p h t", h=4), in1=u_sb[:], op=ALU.mult),
                reads=[pak, "u_sb"], writes=["og_gT"])
            yield

        def swa(s, b):
            first = (s % NSB_SEQ == 0 and b == 0)
            bs = slice(b * 128, (b + 1) * 128)
            pv = []
            dn = []
            for kv in range(2):
                ks_rows = slice(kv * 64, kv * 64 + 64)
                for pc in range(2):
                    if pc == 0 and first:
                        continue
                    bank = 2 + pc
                    if pc == 0:
                        if b == 0:
                            kl, kr = ks_carry[:, kv, :], "ks_carry"
                        else:
                            kl, kr = ksz[:, kv, (b - 1) * 128:b * 128], "ksT"
                    else:
                        kl, kr = ksz[:, kv, bs], "ksT"
                    S.add("pe", lambda e, bank=bank, kl=kl: e.matmul(
                        PS[bank][:, :], lhsT=kl, rhs=qsT[:, :, bs], start=True, stop=False),
                        reads=[kr, "qsT"], writes=["ps%d" % bank])
                    S.add("pe", lambda e, bank=bank, kv=kv, pc=pc: e.matmul(
                        PS[bank][:, :], lhsT=ident[:], rhs=mb[:, kv * 2 + pc, :], start=False, stop=True),
                        reads=["ident", "mb"], writes=["ps%d" % bank])
                    yield
                    pk = "p.%d" % (kv * 2 + pc)
                    S.add("act", lambda e, bank=bank, kv=kv, pc=pc: e.activation(
                        out=p_sb[:, kv * 2 + pc, :], in_=PS[bank][:, :], func=AF.Exp),
                        reads=["ps%d" % bank], writes=[pk])
                    yield
                    if pc == 0:
                        if b == 0:
                            vl, vr = vs_carry[:, kv * 128:(kv + 1) * 128], "vs_carry"
                        else:
                            vl, vr = vs_pad[:, b - 1, kv * 128:(kv + 1) * 128], "vs_pad.%d" % (b - 1)
                    else:
                        vl, vr = vs_pad[:, b, kv * 128:(kv + 1) * 128], "vs_pad.%d" % b
                    pv.append((vl, p_sb[:, kv * 2 + pc, :], [vr, pk]))
                    dn.append((Emat[:, kv, :], p_sb[:, kv * 2 + pc, :], ["Emat", pk]))
            dn.append((selT[32:36, 0:128], smallT[32:36, :], ["sel", "sel2", "sink_hi", "sink_lo"]))
            for i, (l, r, rd) in enumerate(pv):
                S.add("pe", lambda e, l=l, r=r, i=i: e.matmul(
                    PS[4][:, :], lhsT=l, rhs=r, start=(i == 0), stop=(i == len(pv) - 1)),
                    reads=rd, writes=["ps4"])
            yield
            for i, (l, r, rd) in enumerate(dn):
                S.add("pe", lambda e, l=l, r=r, i=i: e.matmul(
                    PS[5][:, :], lhsT=l, rhs=r, start=(i == 0), stop=(i == len(dn) - 1)),
                    reads=rd, writes=["ps5"])
            yield
            S.add("act", lambda e: e.activation(out=rden[:], in_=PS[5][:, :], func=AF.Ln), reads=["ps5"], writes=["rden"])
            yield
            S.add("act", lambda e: e.activation(out=rden[:], in_=rden[:], func=AF.Exp, scale=-1.0),
                  reads=["rden"], writes=["rden"])
            yield
            S.add("pool", lambda e: e.tensor_tensor(
                out=t_sb[:], in0=rden[:].rearrange("p (g t) -> p g t", g=4), in1=szs[:, :, bs], op=ALU.mult),
                reads=["rden", "szs"], writes=["t_sb"])
            yield
            S.add("dve", lambda e: e.tensor_tensor(
                out=os_gT[:, :, bs], in0=PS[4][:, :].rearrange("p (g t) -> p g t", g=4), in1=t_sb[:], op=ALU.mult),
                reads=["ps4", "t_sb"], writes=["os_gT"])
            yield
            if b == 3:
                S.add("pool", lambda e: e.tensor_copy(out=ks_carry[:], in_=ksz[:, :, 384:512]),
                      reads=["ksT"], writes=["ks_carry"])
                S.add("pool", lambda e: e.tensor_copy(out=vs_carry[:], in_=vs_pad[:, 3, :]),
                      reads=["vs_pad.3"], writes=["vs_carry"])
                yield

        def phase3(s):
            for j in range(8):
                if j % 2 == 0:
                    bga, bgb, bys, byg = 2, 3, 0, 7
                else:
                    bga, bgb, bys, byg = 4, 5, 1, 6
                kga, kgb, kys, kyg = pskeys(bga), pskeys(bgb), pskeys(bys), pskeys(byg)
                ca = C_GATE + j * 128
                cb = C_GATE + 1024 + j * 128
                for kc in range(8):
                    S.add("pe", lambda e, kc=kc, ca=ca, bga=bga: e.matmul(
                        PS[bga][:, :], lhsT=w_in_sb[:, kc, ca:ca + 128], rhs=xT[:, kc, :],
                        start=(kc == 0), stop=(kc == 7)),
                        reads=[wgrp(ca)] + XT_ALL, writes=kga)
                for kc in range(8):
                    S.add("pe", lambda e, kc=kc, cb=cb, bgb=bgb: e.matmul(
                        PS[bgb][:, :], lhsT=w_in_sb[:, kc, cb:cb + 128], rhs=xT[:, kc, :],
                        start=(kc == 0), stop=(kc == 7)),
                        reads=[wgrp(cb)] + XT_ALL, writes=kgb)
                for kc in range(4):
                    S.add("pe", lambda e, kc=kc, j=j, bys=bys: e.matmul(
                        PS[bys][:, :], lhsT=w_os_sb[:, kc, j * 128:(j + 1) * 128], rhs=os_gT[:, kc, :],
                        start=(kc == 0), stop=(kc == 3)),
                        reads=["w_os", "os_gT"], writes=kys)
                for kc in range(4):
                    S.add("pe", lambda e, kc=kc, j=j, byg=byg: e.matmul(
                        PS[byg][:, :], lhsT=w_og_sb[:, kc, j * 128:(j + 1) * 128], rhs=og_gT[:, kc, :],
                        start=(kc == 0), stop=(kc == 3)),
                        reads=["w_og", "og_gT"], writes=kyg)
                S.add("act", lambda e, j=j, bga=bga: e.activation(
                    out=ga_sb[:], in_=PS[bga][:, :], func=AF.Sigmoid, bias=bgate[:, j:j + 1]),
                    reads=kga + ["bgate"], writes=["ga_sb"])
                S.add("act", lambda e, j=j, bgb=bgb: e.activation(
                    out=gb_sb[:], in_=PS[bgb][:, :], func=AF.Sigmoid, bias=bgate[:, 8 + j:9 + j]),
                    reads=kgb + ["bgate"], writes=["gb_sb"])
                S.add("dve", lambda e, bys=bys: e.tensor_tensor(out=t2, in0=PS[bys][:, :], in1=gb_sb[:], op=ALU.mult),
                      reads=kys + ["gb_sb"], writes=["t_sb"])
                S.add("dve", lambda e, byg=byg: e.tensor_tensor(out=t1, in0=PS[byg][:, :], in1=ga_sb[:], op=ALU.mult),
                      reads=kyg + ["ga_sb"], writes=["u_sb"])
                S.add("pool", lambda e, j=j: e.tensor_tensor(out=mT[:, j, :], in0=t1, in1=t2, op=ALU.add),
                      reads=["u_sb", "t_sb"], writes=["mT"])

        def phase4(s, b):
            gb = s * 4 + b
            slot = gb % 2
            xk = "xr%d" % slot
            X = xr[slot]
            dma("sp", "xrl%d" % slot, X[:], x_d[gb * 128:(gb + 1) * 128, :], writes=[xk])
            yield
            for half in range(2):
                hsl = slice(half * 512, (half + 1) * 512)
                pb = 4 + half
                for kc in range(8):
                    S.add("pe", lambda e, kc=kc, hsl=hsl, pb=pb: e.matmul(
                        PS[pb][:, :], lhsT=mT[:, kc, b * 128:(b + 1) * 128], rhs=w_out_sb[:, kc, hsl],
                        start=(kc == 0), stop=(kc == 7)),
                        reads=["mT", "w_out"], writes=["ps%d" % pb])
                yield
                S.add("dve", lambda e, hsl=hsl, pb=pb: e.scalar_tensor_tensor(
                    out=X[:, hsl], in0=X[:, hsl], scalar=ALPHA, in1=PS[pb][:, :], op0=ALU.mult, op1=ALU.add),
                    reads=["ps%d" % pb, xk], writes=[xk])
                yield
            S.add("act", lambda e: e.activation(out=X[:], in_=X[:], func=AF.Identity, accum_out=st12[:, 0:1]),
                  reads=[xk], writes=[xk, "st1"])
            S.add("act", lambda e: e.activation(out=junk[:], in_=X[:], func=AF.Square, accum_out=st12[:, 1:2]),
                  reads=[xk], writes=["rs", "st2"])
            yield
            S.add("dve", lambda e: e.tensor_scalar(out=ms12[:], in0=st12[:], scalar1=1.0 / D, scalar2=None, op0=ALU.mult),
                  reads=["st1", "st2"], writes=["ms12"])
            S.add("dve", lambda e: e.scalar_tensor_tensor(
                out=nvar[:], in0=ms12[:, 0:1], scalar=ms12[:, 0:1], in1=ms12[:, 1:2], op0=ALU.mult, op1=ALU.subtract),
                reads=["ms12"], writes=["nvar"])
            yield
            S.add("dve", lambda e: e.tensor_scalar(out=vpe[:], in0=nvar[:], scalar1=-1.0, scalar2=LN_EPS,
                                                   op0=ALU.mult, op1=ALU.add),
                  reads=["nvar"], writes=["vpe"])
            S.add("pool", lambda e: e.tensor_tensor(out=rstd[:], in0=vpe[:], in1=mhalf[:], op=ALU.pow),
                  reads=["vpe", "mhalf"], writes=["rstd"])
            yield
            S.add("dve", lambda e: e.scalar_tensor_tensor(
                out=nbias[:], in0=ms12[:, 0:1], scalar=-1.0, in1=rstd[:], op0=ALU.mult, op1=ALU.mult),
                reads=["ms12", "rstd"], writes=["nbias"])
            yield
            S.add("act", lambda e: e.activation(out=X[:], in_=X[:], func=AF.Identity, scale=rstd[:, 0:1], bias=nbias[:, 0:1]),
                  reads=[xk, "rstd", "nbias"], writes=[xk])
            yield
            S.add("pool", lambda e: e.tensor_tensor(out=X[:], in0=X[:], in1=lng[:], op=ALU.mult),
                  reads=[xk, "lng"], writes=[xk])
            yield
            S.add("pool", lambda e: e.tensor_tensor(out=X[:], in0=X[:], in1=lnb[:], op=ALU.add),
                  reads=[xk, "lnb"], writes=[xk])
            yield
            dma("sp", "st%d" % slot, out_d[gb * 128:(gb + 1) * 128, :], X[:], reads=[xk])
            yield

        def staggered(groups):
            state = []
            for grp in groups:
                gens, lag = grp[0], grp[1]
                init = list(grp[2]) if len(grp) > 2 else [0] * len(gens)
                state.append({"gens": list(gens), "lag": lag, "steps": init, "done": [False] * len(gens)})
            while True:
                progressed = False
                for st in state:
                    for k, g in enumerate(st["gens"]):
                        if st["done"][k]:
                            continue
                        if k > 0 and not st["done"][k - 1] and st["steps"][k - 1] < st["lag"]:
                            continue
                        try:
                            next(g)
                            st["steps"][k] += 1
                        except StopIteration:
                            st["done"][k] = True
                        progressed = True
                if not progressed:
                    break

        def timed(items):
            st = [[g, f, n0, False] for g, f, n0 in items]
            while True:
                best = None
                for k, (g, f, n, done) in enumerate(st):
                    if done:
                        continue
                    t = f(n + 1)
                    if best is None or t < best[0]:
                        best = (t, k)
                if best is None:
                    break
                k = best[1]
                try:
                    next(st[k][0])
                    st[k][2] += 1
                except StopIteration:
                    st[k][3] = True

        def delayed_prefix(gen, delay, nsteps):
            for _ in range(delay):
                yield
            for _ in range(nsteps):
                try:
                    next(gen)
                except StopIteration:
                    return
                yield

        HOIST = int(os.environ.get("KERNEL_HOIST", "8"))
        GL_LAG = int(os.environ.get("KERNEL_GL_LAG", "7"))
        SW_LAG = int(os.environ.get("KERNEL_SW_LAG", "10"))
        P4_LAG = int(os.environ.get("KERNEL_P4_LAG", "7"))
        FRONT_EARLY = int(os.environ.get("KERNEL_FRONT_EARLY", "1"))
        SCHED = int(os.environ.get("KERNEL_SCHED", "2"))
        SP = float(os.environ.get("KERNEL_SP", "14"))
        SF0 = float(os.environ.get("KERNEL_SF0", "6"))
        SFS = float(os.environ.get("KERNEL_SFS", "1.2"))
        SB0 = float(os.environ.get("KERNEL_SB0", "1.5"))
        SBS = float(os.environ.get("KERNEL_SBS", "1.0"))
        SSS = float(os.environ.get("KERNEL_SSS", "1.0"))
        prev = None
        for idx, s in enumerate(sb_list):
            nxt = sb_list[idx + 1] if idx + 1 < len(sb_list) else None
            phase0(s, preloaded=(0, 1, 2, 3), nxt=nxt)
            g_gla = [gla(s, b) for b in range(4)]
            g_swa = [swa(s, b) for b in range(4)]
            p4s = [phase4(prev, b) for b in range(4)] if prev is not None else []
            groups = [([phase1(s)], 0)]
            if p4s:
                groups.append((p4s, P4_LAG))
            if HOIST > 0:
                groups.append(([delayed_prefix(g_gla[0], 11, HOIST)], 0))
            staggered(groups)
            if SCHED == 0:
                def t_gla(b, i):
                    return b * GL_LAG + i - (FRONT_EARLY if (i <= 8 and b > 0) else 0) + 0.5
                def t_swa(b, i):
                    return b * SW_LAG + i + 0.25
            elif SCHED == 2:
                FRONT_T = [3.2, 3.3, 3.4, 8.2, 8.3, 8.4, 11.8, 11.9]
                BACK_T = [1.5, 2.5, 7.5, 7.6, 8.5, 8.6, 9.5, 11.5, 12.5, 13.5, 14.5, 15.5]
                def t_gla(b, i):
                    if i <= 8:
                        return 14.0 * (b - 1) + FRONT_T[i - 1]
                    return 14.0 * b + (BACK_T[i - 9] if i <= 20 else BACK_T[-1] + (i - 20))
                def t_swa(b, i):
                    return 14.0 * b + i
            else:
                def t_gla(b, i):
                    if i <= 8:
                        return SP * (b - 1) + SF0 + (i - 1) * SFS
                    return SP * b + SB0 + (i - 9) * SBS
                def t_swa(b, i):
                    return SP * b + i * SSS
            timed([(g_gla[b], (lambda i, b=b: t_gla(b, i)), (HOIST if b == 0 else 0)) for b in range(4)] +
                  [(g_swa[b], (lambda i, b=b: t_swa(b, i)), 0) for b in range(4)])
            phase3(s)
            prev = s
        staggered([([phase4(prev, b) for b in range(4)], P4_LAG)])

        S.finalize()
        sems = {}
        for key in S.sem_keys:
            sems[key] = es.enter_context(nc.semaphore("s_%s_%s" % key))
        final_waits = [k for k in S.sem_keys if k[0] == "dma" and k[1].startswith("st")]
        with nc.Block() as block:
            S.emit(nc, block, sems, final_waits)
    return nc


def _consts():
    f = np.float32
    ident = np.eye(128, dtype=f)
    m = np.arange(128)
    tri = (m[:, None] <= m[None, :]).astype(f)
    mask = np.tile(tri, (1, 4))
    mbm = np.zeros((128, 4, 4, 128), dtype=f)
    s_ = m[:, None]
    q_ = m[None, :]
    NEG = -30000.0
    for kv in range(2):
        for g in range(4):
            slope = 2.0 ** (-(kv * 4 + g + 1))
            dist_prev = (q_ + 128 - s_).astype(f)
            dist_cur = (q_ - s_).astype(f)
            mbm[:, kv * 2 + 0, g, :] = np.where(s_ > q_, -slope * dist_prev, NEG)
            mbm[:, kv * 2 + 1, g, :] = np.where(s_ <= q_, -slope * dist_cur, NEG)
    E = np.zeros((128, 2, 128), dtype=f)
    E[:, 0, 0:64] = 1.0
    E[:, 1, 64:128] = 1.0
    sel = np.zeros((2, 128), dtype=f)
    sel[0, 0:64] = 1.0
    sel[1, 64:128] = 1.0
    gk = np.zeros((16, 512), dtype=f)
    gk[0, :] = 1.0
    return dict(c_ident=ident, c_mask=np.ascontiguousarray(mask),
                c_mb=np.ascontiguousarray(mbm.reshape(128, 2048)),
                c_E=np.ascontiguousarray(E.reshape(128, 256)), c_sel=sel, c_gk=gk)


def _pair_perm():
    idx = []
    for j in range(4):
        idx.extend(range(j * 64, j * 64 + 64))
        idx.extend(range((4 + j) * 64, (4 + j) * 64 + 64))
    return np.array(idx)


_NC_CACHE = {}


def kernel(x, w_in, w_gk2, b_gk, gla_norm_g, w_o_gla, sinks, w_o_swa, b_gate, w_out, ln_g, ln_b):
    f = np.float32
    nsb = int(os.environ.get("KERNEL_NSB", "16"))
    sb_list = list(range(nsb))
    x = np.asarray(x, dtype=f)
    perm = _pair_perm()
    w_in_p = np.array(w_in, dtype=f, copy=True)
    w_in_p[:, C_QS:C_QS + 512] = np.asarray(w_in)[:, C_QS + perm]
    w_in_p[:, C_ZS:C_ZS + 512] = np.asarray(w_in)[:, C_ZS + perm]
    w_os_p = np.ascontiguousarray(np.asarray(w_o_swa, dtype=f)[perm, :])
    wgk_aug = np.zeros((32, 256), dtype=f)
    wgk_aug[0:16] = np.asarray(w_gk2, dtype=f)
    wgk_aug[16] = np.asarray(b_gk, dtype=f)
    shared = dict(
        w_in=np.ascontiguousarray(w_in_p), w_o_gla=np.ascontiguousarray(np.asarray(w_o_gla, dtype=f)),
        w_o_swa=w_os_p, w_out=np.ascontiguousarray(np.asarray(w_out, dtype=f)), wgk_aug=wgk_aug,
        gnorm=np.ascontiguousarray(np.asarray(gla_norm_g, dtype=f).reshape(128, 1)),
        sinks_rep=np.ascontiguousarray(np.repeat(np.asarray(sinks, dtype=f).reshape(2, 4), 128, axis=1)),
        bgate=np.ascontiguousarray(np.asarray(b_gate, dtype=f).reshape(16, 128).T),
        lng_bc=np.ascontiguousarray(np.broadcast_to(np.asarray(ln_g, dtype=f)[None, :], (128, D))),
        lnb_bc=np.ascontiguousarray(np.broadcast_to(np.asarray(ln_b, dtype=f)[None, :], (128, D))),
    )
    shared.update(_consts())
    key = tuple(sb_list)
    if key not in _NC_CACHE:
        _NC_CACHE[key] = build_nc(sb_list)
    nc = _NC_CACHE[key]
    xs = x.reshape(N_CORES, NTOK, D)
    in_maps = []
    for c in range(N_CORES):
        m = dict(shared)
        m["x"] = np.ascontiguousarray(xs[c])
        in_maps.append(m)
    res = run_bass_kernel_spmd(nc, in_maps, core_ids=list(range(N_CORES)))
    outs = [np.asarray(r["out"], dtype=f).reshape(SEQ_PER_CORE, SEQ, D) for r in res.results]
    return np.concatenate(outs, axis=0).astype(f)
```

```python
import os
from contextlib import ExitStack

import numpy as np
import concourse.bass as bass
import concourse.mybir as mybir
from concourse.bass_utils import run_bass_kernel_spmd

F32 = mybir.dt.float32
BF16 = mybir.dt.bfloat16
AF = mybir.ActivationFunctionType
ALU = mybir.AluOpType

N_CORES = 8
D = 1024
SEQ = 2048
SEQ_PER_CORE = 4
NTOK = SEQ * SEQ_PER_CORE
T = 512
NSB_SEQ = SEQ // T
D_IN = 4880
ALPHA = 2.0 ** 0.25
LN_EPS = 1e-5
RMS_EPS = 1e-6

C_QG, C_KG, C_VG, C_GK, C_ZG, C_QS, C_KS, C_VS, C_ZS, C_GATE = (
    0, 256, 512, 1024, 1040, 1552, 2064, 2192, 2320, 2832)
W_GROUPS = [(0, 512), (512, 1040), (1040, 1552), (1552, 2064), (2064, 2320),
            (2320, 2832), (2832, 3344), (3344, 3856), (3856, 4368), (4368, 4880)]


def pskeys(bank):
    return ["ps%d" % bank]


def wgrp(c):
    for i, (a, b) in enumerate(W_GROUPS):
        if a <= c < b:
            return "w_in.%d" % i
    raise ValueError(c)


class _Op:
    __slots__ = ("eng", "fn", "raw", "oth", "dma", "need_inc", "done", "waits")

    def __init__(self, eng, fn, raw, oth, dma):
        self.eng, self.fn, self.raw, self.oth, self.dma = eng, fn, raw, oth, dma
        self.need_inc = False
        self.done = None
        self.waits = []


class Sched:
    ENGS = ("pe", "act", "dve", "pool", "sp")

    def __init__(self):
        self.ops = []
        self.last_w = {}
        self.readers = {}

    def add(self, eng, fn, reads=(), writes=(), dma=None):
        i = len(self.ops)
        raw, oth = set(), set()
        for r in reads:
            w = self.last_w.get(r)
            if w is not None:
                raw.add(w)
        for k in writes:
            w = self.last_w.get(k)
            if w is not None:
                raw.add(w)
            for rd in self.readers.get(k, ()):
                oth.add(rd)
        for r in reads:
            self.readers.setdefault(r, []).append(i)
        for k in writes:
            self.last_w[k] = i
            self.readers[k] = []
        self.ops.append(_Op(eng, fn, raw, oth - raw, dma))
        return i

    def finalize(self):
        ops = self.ops
        for op in ops:
            deps = set()
            for d in op.raw:
                p = ops[d]
                deps.add(d)
            for d in op.oth:
                p = ops[d]
                if p.eng == op.eng and p.dma is None:
                    continue
                deps.add(d)
            if op.eng == "pe":
                deps = {d for d in deps if not (ops[d].eng == "pe" and ops[d].dma is None)}
            op.raw = deps
            for d in deps:
                ops[d].need_inc = True
        cnt = {}
        self.sem_keys = []
        for op in ops:
            if op.dma is not None:
                key = ("dma", op.dma)
                cnt[key] = cnt.get(key, 0) + 16
                op.done = (key, cnt[key])
            elif op.need_inc:
                key = ("eng", op.eng)
                cnt[key] = cnt.get(key, 0) + 1
                op.done = (key, cnt[key])
            else:
                continue
            if key not in self.sem_keys:
                self.sem_keys.append(key)
        self.final_cnt = cnt
        seen = {e: {} for e in self.ENGS}
        for op in ops:
            w = {}
            for d in op.raw:
                key, c = ops[d].done
                if w.get(key, 0) < c:
                    w[key] = c
            sw = seen[op.eng]
            op.waits = []
            for key, c in w.items():
                if sw.get(key, 0) < c:
                    op.waits.append((key, c))
                    sw[key] = c

    def emit(self, nc, block, sems, final_waits):
        reg = {"pe": block.tensor, "act": block.scalar, "dve": block.vector,
               "pool": block.gpsimd, "sp": block.sync}
        for eng in self.ENGS:
            ops_e = [op for op in self.ops if op.eng == eng]

            def body(e, ops_e=ops_e, eng=eng):
                for op in ops_e:
                    for key, c in op.waits:
                        e.wait_ge(sems[key], c)
                    ins = op.fn(e)
                    if op.dma is not None:
                        ins.then_inc(sems[("dma", op.dma)], 16)
                    elif op.need_inc:
                        ins.then_inc(sems[("eng", eng)], 1)
                if eng == "sp":
                    for key in final_waits:
                        e.wait_ge(sems[key], self.final_cnt[key])

            reg[eng](body)


def _interleave(gens):
    gens = list(gens)
    while gens:
        for g in list(gens):
            try:
                next(g)
            except StopIteration:
                gens.remove(g)


def build_nc(sb_list):
    nc = bass.Bass("TRN2", target_bir_lowering=False)

    def din(name, shape, dt=F32):
        return nc.dram_tensor(name, list(shape), dt, kind="ExternalInput").ap()

    x_d = din("x", [NTOK, D])
    w_in_d = din("w_in", [D, D_IN])
    w_og_d = din("w_o_gla", [512, D])
    w_os_d = din("w_o_swa", [512, D])
    w_out_d = din("w_out", [D, D])
    wgk_d = din("wgk_aug", [32, 256])
    gnorm_d = din("gnorm", [128, 1])
    sinks_d = din("sinks_rep", [2, 512])
    bgate_d = din("bgate", [128, 16])
    lng_d = din("lng_bc", [128, D])
    lnb_d = din("lnb_bc", [128, D])
    c_ident_d = din("c_ident", [128, 128])
    c_mask_d = din("c_mask", [128, 512])
    c_mb_d = din("c_mb", [128, 2048])
    c_E_d = din("c_E", [128, 256])
    c_sel_d = din("c_sel", [2, 128])
    c_gk_d = din("c_gk", [16, 512])
    out_d = nc.dram_tensor("out", [NTOK, D], F32, kind="ExternalOutput").ap()

    S = Sched()
    es = ExitStack()

    def sb(name, shape, dt):
        return es.enter_context(nc.sbuf_tensor(name, list(shape), dt))

    with es:
        w_in_sb = sb("w_in_sb", [128, 8, D_IN], BF16)
        w_og_sb = sb("w_og_sb", [128, 4, D], BF16)
        w_os_sb = sb("w_os_sb", [128, 4, D], BF16)
        w_out_sb = sb("w_out_sb", [128, 8, D], BF16)
        ident = sb("ident", [128, 128], BF16)
        tri = sb("tri", [128, 128], F32)
        mb = sb("mb", [128, 4, 512], BF16)
        ones = sb("ones", [128, 128], BF16)
        Emat = sb("Emat", [128, 2, 128], BF16)
        wgk_t = sb("wgk", [36, 256], BF16)
        wgk = wgk_t[0:32, :]
        selT = wgk_t
        gnorm = sb("gnorm_sb", [128, 1], F32)
        bgate = sb("bgate_sb", [128, 16], F32)
        lng = sb("lng", [128, D], F32)
        lnb = sb("lnb", [128, D], F32)

        xbf = [sb("xbf%d" % i, [128, D], BF16) for i in range(4)]
        xT = sb("xT", [128, 8, T], BF16)
        qgT = sb("qgT", [128, 2, T], BF16)
        kgT = sb("kgT", [128, 2, T], BF16)
        szg = sb("szg", [128, 4, T], BF16)
        qsT = sb("qsT", [128, 4, T], BF16)
        ksz = sb("ksz", [128, 2, T], BF16)
        szs = sb("szs", [128, 4, T], BF16)
        smallT = sb("smallT", [128, T], BF16)
        gkT = smallT[0:32, :]
        sink_hi = smallT[32:34, :]
        sink_lo = smallT[34:36, :]
        sel_hi = selT[32:34, 0:128]
        sel_lo = selT[34:36, 0:128]
        vg_tok = sb("vg_tok", [128, 4, 512], BF16)
        vs_pad = sb("vs_pad", [128, 4, 256], BF16)
        ks_carry = sb("ks_carry", [128, 2, 128], BF16)
        vs_carry = sb("vs_carry", [128, 256], BF16)

        sp_sb = sb("sp_sb", [128, 256], F32)
        eGi = sb("eGi", [128, 2, 128], F32)
        dec = [sb("dec%d" % i, [128, 2, 1], F32) for i in range(4)]
        decm = sb("decm", [128, 2, 2, 1], F32)
        halfmask = sb("halfmask", [128, 2, 2, 1], F32)
        mhalf = sb("mhalf", [128, 1], F32)
        vpe = sb("vpe", [128, 1], F32)
        q_dec = sb("q_dec", [128, 2, 128], BF16)
        kinv_tok = sb("kinv_tok", [128, 256], BF16)
        k_inv = kinv_tok[:].rearrange("p (c t) -> p c t", c=2)
        AT_m = sb("AT_m", [128, 512], BF16)
        Rst = sb("Rst", [128, 2, 128], F32)
        Sz = sb("Sz", [128, 2, 2, 128], BF16)
        kz = sb("kz", [128, 2, 2, 128], BF16)
        rs = sb("rs", [128, 512], F32)
        u_sb = sb("u_sb", [128, 4, 128], BF16)
        p_sb = sb("p_sb", [128, 4, 512], BF16)
        rden = sb("rden", [128, 512], F32)
        t_sb = sb("t_sb", [128, 4, 128], BF16)
        og_gT = sb("og_gT", [128, 4, T], BF16)
        os_gT = sb("os_gT", [128, 4, T], BF16)

        ga_sb = sb("ga_sb", [128, T], BF16)
        gb_sb = sb("gb_sb", [128, T], BF16)
        mT = sb("mT", [128, 8, T], BF16)

        xr = [sb("xr%d" % i, [128, D], F32) for i in range(2)]
        st12 = sb("st12", [128, 2], F32)
        ms12 = sb("ms12", [128, 2], F32)
        nvar = sb("nvar", [128, 1], F32)
        rstd = sb("rstd", [128, 1], F32)
        nbias = sb("nbias", [128, 1], F32)

        eG = sp_sb[:].rearrange("p (c t) -> p c t", c=2)
        t1 = u_sb[:].rearrange("p h t -> p (h t)")
        t2 = t_sb[:].rearrange("p h t -> p (h t)")
        PS = [es.enter_context(nc.psum_tensor("ps%d" % i, [128, 512], F32)) for i in range(8)]
        PSb7 = PS[7].bitcast(BF16)
        PSb0 = PS[0].bitcast(BF16)
        junk = rs.bitcast(BF16)

        def dma(eng, key, out, in_, reads=(), writes=()):
            S.add(eng, lambda e, o=out, i=in_: e.dma_start(out=o, in_=i),
                  reads=reads, writes=writes, dma=key)

        w_in_v = w_in_d.rearrange("(kc p) n -> p kc n", p=128)
        dma("pool", "c_ident", ident[:], c_ident_d, writes=["ident"])
        dma("pool", "c_gk", smallT[16:32, :], c_gk_d, writes=["gkT.c"])
        def wload(gi):
            a, b = W_GROUPS[gi]
            dma("pool", "w_in.%d" % gi, w_in_sb[:, :, a:b], w_in_v[:, :, a:b], writes=["w_in.%d" % gi])

        first_s = sb_list[0]
        for b in range(4):
            gb0 = first_s * 4 + b
            dma("pool", "xbf%d" % b, xbf[b][:], x_d[gb0 * 128:(gb0 + 1) * 128, :], writes=["xbf%d" % b])
        wload(0)
        wload(1)
        dma("pool", "c_wgk", wgk, wgk_d, writes=["wgk"])
        dma("sp", "c_tri", tri[:], c_mask_d[:, 0:128], writes=["tri"])
        dma("sp", "c_gnorm", gnorm[:], gnorm_d, writes=["gnorm"])
        sink_f = rden[0:2, :]
        sink_hi32 = rs[0:2, :]
        dma("sp", "c_sinks", sink_f, sinks_d, writes=["rden"])
        dma("sp", "c_bgate", bgate[:], bgate_d, writes=["bgate"])
        dma("sp", "c_lng", lng[:], lng_d, writes=["lng"])
        dma("sp", "c_lnb", lnb[:], lnb_d, writes=["lnb"])
        for gi in (3, 4, 2, 5):
            wload(gi)
        dma("pool", "c_mb", mb[:].rearrange("p a b -> p (a b)"), c_mb_d, writes=["mb"])
        dma("pool", "c_E", Emat[:].rearrange("p a b -> p (a b)"), c_E_d, writes=["Emat"])
        dma("pool", "c_sel", sel_hi, c_sel_d, writes=["sel"])
        dma("pool", "c_sel2", sel_lo, c_sel_d, writes=["sel2"])
        for gi in (6, 8):
            wload(gi)
        dma("pool", "w_os", w_os_sb[:], w_os_d.rearrange("(kc p) n -> p kc n", p=128), writes=["w_os"])
        dma("pool", "w_og", w_og_sb[:], w_og_d.rearrange("(kc p) n -> p kc n", p=128), writes=["w_og"])
        for gi in (7, 9):
            wload(gi)
        dma("pool", "w_out", w_out_sb[:], w_out_d.rearrange("(kc p) n -> p kc n", p=128), writes=["w_out"])

        S.add("dve", lambda e: e.memset(ones[:], 1.0), writes=["ones"])
        S.add("dve", lambda e: e.memset(vs_pad[:], 0.0), writes=["vs_pad.%d" % b for b in range(4)])
        S.add("dve", lambda e: e.memset(vs_carry[:], 0.0), writes=["vs_carry"])
        S.add("dve", lambda e: e.memset(ksz[:], 0.0), writes=["ksT"])
        S.add("dve", lambda e: e.memset(ks_carry[:], 0.0), writes=["ks_carry"])
        S.add("dve", lambda e: e.memset(Sz[:], 0.0), writes=["Sbf"])
        S.add("dve", lambda e: e.memset(kz[:], 0.0), writes=["kz"])
        S.add("dve", lambda e: e.memset(mhalf[:], -0.5), writes=["mhalf"])
        S.add("dve", lambda e: e.memset(halfmask[:], 0.0), writes=["halfmask"])
        S.add("dve", lambda e: e.memset(halfmask[0:64, :, 0, :], 1.0), writes=["halfmask"])
        S.add("dve", lambda e: e.memset(halfmask[64:128, :, 1, :], 1.0), writes=["halfmask"])
        S.add("act", lambda e: e.activation(out=sink_f, in_=sink_f, func=AF.Exp),
              reads=["rden"], writes=["rden"])
        sink_t = p_sb[0:2, 0:2, :]
        S.add("dve", lambda e: e.tensor_copy(out=sink_t[:, 0, :], in_=sink_f), reads=["rden"], writes=["p.0"])
        S.add("dve", lambda e: e.tensor_copy(out=sink_hi32, in_=sink_t[:, 0, :]), reads=["p.0"], writes=["rs"])
        S.add("dve", lambda e: e.tensor_tensor(out=sink_t[:, 1, :], in0=sink_f, in1=sink_hi32, op=ALU.subtract),
              reads=["rden", "rs"], writes=["p.1"])
        dma("sp", "c_sh", sink_hi, sink_t[:, 0, :], reads=["p.0"], writes=["sink_hi"])
        dma("sp", "c_sl", sink_lo, sink_t[:, 1, :], reads=["p.1"], writes=["sink_lo"])

        W_ALL = ["w_in.%d" % i for i in range(10)]
        XT_ALL = ["xT.%d" % b for b in range(4)]

        def emit_xload(s, b):
            gb = s * 4 + b
            slot = b
            dma("pool", "xbf%d" % slot, xbf[slot][:], x_d[gb * 128:(gb + 1) * 128, :],
                writes=["xbf%d" % slot])

        def phase0(s, preloaded, nxt=None):
            for b in range(4):
                if b not in preloaded:
                    emit_xload(s, b)
                slot = b
                pbk, pkey = ((PSb7, "ps7"), (PSb0, "ps0"))[b % 2]
                for kc in range(8):
                    S.add("pe", lambda e, kc=kc, slot=slot, pbk=pbk: e.transpose(
                        out=pbk[:, kc * 128:(kc + 1) * 128], in_=xbf[slot][:, kc * 128:(kc + 1) * 128],
                        identity=ident[:]),
                        reads=["xbf%d" % slot, "ident"], writes=[pkey])
                S.add(("dve", "act")[b % 2], lambda e, b=b, pbk=pbk: (
                    e.tensor_copy(out=xT[:, :, b * 128:(b + 1) * 128],
                                  in_=pbk[:].rearrange("p (k t) -> p k t", k=8)) if b % 2 == 0 else
                    e.activation(out=xT[:, :, b * 128:(b + 1) * 128],
                                 in_=pbk[:].rearrange("p (k t) -> p k t", k=8), func=AF.Copy)),
                    reads=[pkey], writes=["xT.%d" % b])
                if nxt is not None:
                    emit_xload(nxt, b)

        def phase1(s):
            chunks = []
            for c in range(2):
                chunks.append(("qg", c, C_QG + c * 128, 128))
            for c in range(2):
                chunks.append(("kg", c, C_KG + c * 128, 128))
            chunks.append(("gk", 0, C_GK, 128))
            for c in range(4):
                chunks.append(("qs", c, C_QS + c * 128, 128))
            chunks.append(("ks", 0, C_KS, 128))
            for c in range(4):
                chunks.append(("zg", c, C_ZG + c * 128, 128))
            for c in range(4):
                chunks.append(("zs", c, C_ZS + c * 128, 128))
            for i, (kind, c, c0, M) in enumerate(chunks):
                if i > 0:
                    yield
                bank = (1, 6, 2, 3)[i % 4] if i < 14 else (1, 6)[i % 2]
                for kc in range(8):
                    if kc == 4:
                        yield
                    S.add("pe", lambda e, kc=kc, bank=bank, c0=c0, M=M: e.matmul(
                        PS[bank][0:M, :], lhsT=w_in_sb[:, kc, c0:c0 + M], rhs=xT[:, kc, :],
                        start=(kc == 0), stop=(kc == 7)),
                        reads=sorted({wgrp(c0), wgrp(c0 + M - 1)}) + XT_ALL, writes=pskeys(bank))
                src = PS[bank]
                rd = pskeys(bank)
                if kind == "qg":
                    S.add("dve", lambda e, c=c, src=src: e.tensor_scalar(
                        out=qgT[:, c, :], in0=src[:], scalar1=0.125, scalar2=None, op0=ALU.mult),
                        reads=rd, writes=["qgT"])
                elif kind == "kg":
                    S.add("dve", lambda e, c=c, src=src: e.tensor_copy(out=kgT[:, c, :], in_=src[:]),
                          reads=rd, writes=["kgT"])
                elif kind == "gk":
                    S.add("dve", lambda e, src=src: e.tensor_copy(out=smallT[0:16, :], in_=src[0:16, :]),
                          reads=rd, writes=["gkT"])
                elif kind == "qs":
                    S.add("act", lambda e, c=c, src=src: e.activation(
                        out=qsT[:, c, :], in_=src[:], func=AF.Copy, scale=0.125),
                        reads=rd, writes=["qsT"])
                elif kind == "ks":
                    for kv in range(2):
                        S.add("dve", lambda e, src=src, kv=kv: e.tensor_copy(
                            out=ksz[kv * 64:(kv + 1) * 64, kv, :], in_=src[kv * 64:(kv + 1) * 64, :]),
                            reads=rd, writes=["ksT"])
                elif kind == "zg":
                    S.add("act", lambda e, c=c, src=src: e.activation(
                        out=szg[:, c, :], in_=src[:], func=AF.Silu),
                        reads=rd, writes=["szg"])
                elif kind == "zs":
                    S.add("act", lambda e, c=c, src=src: e.activation(
                        out=szs[:, c, :], in_=src[:], func=AF.Silu),
                        reads=rd, writes=["szs"])
            for b in range(4):
                yield
                for kc in range(8):
                    S.add("pe", lambda e, kc=kc, b=b: e.matmul(
                        PS[2][:, :], lhsT=xT[:, kc, b * 128:(b + 1) * 128], rhs=w_in_sb[:, kc, C_VG:C_VG + 512],
                        start=(kc == 0), stop=(kc == 7)),
                        reads=[wgrp(C_VG), "xT.%d" % b], writes=["ps2"])
                for kc in range(8):
                    S.add("pe", lambda e, kc=kc, b=b: e.matmul(
                        PS[3][:, 0:128], lhsT=xT[:, kc, b * 128:(b + 1) * 128], rhs=w_in_sb[:, kc, C_VS:C_VS + 128],
                        start=(kc == 0), stop=(kc == 7)),
                        reads=[wgrp(C_VS), "xT.%d" % b], writes=["ps3"])
                S.add("dve", lambda e, b=b: e.tensor_copy(out=vg_tok[:, b, :], in_=PS[2][:, :]),
                      reads=["ps2"], writes=["vg_tok.%d" % b])
                for kv in range(2):
                    S.add("act", lambda e, b=b, kv=kv: e.activation(
                        out=vs_pad[:, b, kv * 128 + kv * 64: kv * 128 + kv * 64 + 64],
                        in_=PS[3][:, kv * 64:(kv + 1) * 64], func=AF.Copy),
                        reads=["ps3"], writes=["vs_pad.%d" % b])

        def gla(s, b):
            first = (s % NSB_SEQ == 0 and b == 0)
            gb = s * 4 + b
            dcur, dprev = dec[gb % 4], dec[(gb + 3) % 4]
            dck, dpk = "dec%d" % (gb % 4), "dec%d" % ((gb + 3) % 4)
            bs = slice(b * 128, (b + 1) * 128)
            PA, pak = (PS[1], "ps1") if b % 2 == 0 else (PS[7], "ps7")
            S.add("pe", lambda e: e.matmul(PS[0][:, 0:256], lhsT=smallT[0:32, bs], rhs=wgk, start=True, stop=True),
                  reads=["gkT", "gkT.c", "wgk"], writes=["ps0"])
            yield
            S.add("act", lambda e: e.activation(out=sp_sb[:], in_=PS[0][:, 0:256], func=AF.Exp, scale=-1.0),
                  reads=["ps0"], writes=["sp_sb"])
            yield
            S.add("act", lambda e: e.activation(out=sp_sb[:], in_=sp_sb[:], func=AF.Ln, bias=1.0),
                  reads=["sp_sb"], writes=["sp_sb"])
            yield
            for c in range(2):
                S.add("pe", lambda e, c=c: e.matmul(
                    PS[0][:, 256 + c * 128:256 + (c + 1) * 128], lhsT=sp_sb[:, c * 128:(c + 1) * 128], rhs=tri[:],
                    start=True, stop=True),
                    reads=["sp_sb", "tri"], writes=["ps0"])
            yield
            GTv = PS[0][:, 256:512].rearrange("p (c t) -> p c t", c=2)
            S.add("act", lambda e: e.activation(out=eG, in_=GTv, func=AF.Exp, scale=-1.0 / 16.0),
                  reads=["ps0"], writes=["sp_sb"])
            S.add("act", lambda e: e.activation(out=eGi[:], in_=GTv, func=AF.Exp, scale=1.0 / 16.0),
                  reads=["ps0"], writes=["eGi"])
            S.add("act", lambda e: e.activation(out=dcur[:], in_=GTv[:, :, 127:128], func=AF.Exp, scale=-1.0 / 16.0),
                  reads=["ps0"], writes=[dck])
            yield
            S.add("dve", lambda e: e.tensor_tensor(out=k_inv, in0=kgT[:, :, bs], in1=eGi[:], op=ALU.mult),
                  reads=["kgT", "eGi"], writes=["kinv_tok"])
            S.add("dve", lambda e: e.tensor_tensor(out=q_dec[:], in0=qgT[:, :, bs], in1=eG, op=ALU.mult),
                  reads=["qgT", "sp_sb"], writes=["q_dec"])
            for hh in range(2):
                hs = slice(hh * 64, hh * 64 + 64)
                S.add("dve", lambda e, hh=hh, hs=hs: e.tensor_tensor(
                    out=kz[hs, :, hh, :], in0=kgT[hs, :, bs], in1=eGi[hs, :, :], op=ALU.mult),
                    reads=["kgT", "eGi"], writes=["kz"])
            yield
            for c in range(2):
                S.add("pe", lambda e, c=c: e.transpose(
                    out=PSb0[:, c * 128:(c + 1) * 128], in_=k_inv[:, c, :], identity=ident[:]),
                    reads=["kinv_tok", "ident"], writes=["ps0"])
            yield
            S.add("dve", lambda e: e.tensor_copy(out=kinv_tok[:], in_=PSb0[:, 0:256]),
                  reads=["ps0"], writes=["kinv_tok"])
            yield
            for h in range(4):
                c, hs = h // 2, slice((h % 2) * 64, (h % 2) * 64 + 64)
                S.add("pe", lambda e, h=h, c=c: e.matmul(
                    PA[:, h * 128:(h + 1) * 128], lhsT=kz[:, c, h % 2, :], rhs=q_dec[:, c, :],
                    start=True, stop=True),
                    reads=["kz", "q_dec"], writes=[pak])
            yield
            S.add("dve", lambda e: e.tensor_tensor(
                out=AT_m[:].rearrange("p (h t) -> p h t", h=4), in0=PA[:, :].rearrange("p (h t) -> p h t", h=4),
                in1=tri[:].unsqueeze(1).to_broadcast([128, 4, 128]), op=ALU.mult),
                reads=[pak, "tri"], writes=["AT_m"])
            yield
            for h in range(4):
                c, hs = h // 2, slice((h % 2) * 64, (h % 2) * 64 + 64)
                S.add("pe", lambda e, h=h: e.matmul(
                    PA[:, h * 128:(h + 1) * 128], lhsT=vg_tok[:, b, h * 128:(h + 1) * 128],
                    rhs=AT_m[:, h * 128:(h + 1) * 128], start=True, stop=first),
                    reads=["vg_tok.%d" % b, "AT_m"], writes=[pak])
                if not first:
                    S.add("pe", lambda e, h=h, c=c: e.matmul(
                        PA[:, h * 128:(h + 1) * 128], lhsT=Sz[:, c, h % 2, :], rhs=q_dec[:, c, :],
                        start=False, stop=True),
                        reads=["Sbf", "q_dec"], writes=[pak])
            yield
            for c in range(2):
                S.add("pe", lambda e, c=c: e.matmul(
                    PS[6][:, c * 256:(c + 1) * 256], lhsT=kinv_tok[:, c * 128:(c + 1) * 128],
                    rhs=vg_tok[:, b, c * 256:(c + 1) * 256], start=True, stop=True),
                    reads=["kinv_tok", "vg_tok.%d" % b], writes=["ps6"])
            yield
            for c in range(2):
                for hh in range(2):
                    hs = slice(hh * 64, hh * 64 + 64)
                    src = PS[6][hs, c * 256 + hh * 128: c * 256 + hh * 128 + 128]
                    if first:
                        S.add("dve", lambda e, c=c, hs=hs, src=src: e.tensor_copy(out=Rst[hs, c, :], in_=src),
                              reads=["ps6"], writes=["Rst"])
                    else:
                        S.add("dve", lambda e, c=c, hs=hs, src=src: e.scalar_tensor_tensor(
                            out=Rst[hs, c, :], in0=Rst[hs, c, :], scalar=dprev[hs, c, :], in1=src,
                            op0=ALU.mult, op1=ALU.add),
                            reads=["ps6", "Rst", dpk], writes=["Rst"])
            yield
            S.add("dve", lambda e: e.tensor_tensor(
                out=decm[:], in0=dcur[:].unsqueeze(2).to_broadcast([128, 2, 2, 1]), in1=halfmask[:], op=ALU.mult),
                reads=[dck, "halfmask"], writes=["decm"])
            S.add("dve", lambda e: e.tensor_tensor(
                out=Sz[:], in0=Rst[:].unsqueeze(2).to_broadcast([128, 2, 2, 128]),
                in1=decm[:].to_broadcast([128, 2, 2, 128]), op=ALU.mult),
                reads=["Rst", "decm"], writes=["Sbf"])
            yield
            S.add("act", lambda e: e.activation(out=AT_m[:], in_=PA[:, :], func=AF.Square),
                  reads=[pak], writes=["AT_m"])
            yield
            S.add("pe", lambda e: e.matmul(PS[6][:, :], lhsT=ones[:], rhs=AT_m[:], start=True, stop=True),
                  reads=["ones", "AT_m"], writes=["ps6"])
            yield
            S.add("act", lambda e: e.activation(out=rs[:], in_=PS[6][:, :], func=AF.Ln, scale=1.0 / 128.0, bias=RMS_EPS),
                  reads=["ps6"], writes=["rs"])
            yield
            S.add("act", lambda e: e.activation(out=rs[:], in_=rs[:], func=AF.Exp, scale=-0.5),
                  reads=["rs"], writes=["rs"])
            yield
            S.add("dve", lambda e: e.scalar_tensor_tensor(
                out=u_sb[:], in0=rs[:].rearrange("p (h t) -> p h t", h=4), scalar=gnorm[:, 0:1],
                in1=szg[:, :, bs], op0=ALU.mult, op1=ALU.mult),
                reads=["rs", "gnorm", "szg"], writes=["u_sb"])
            yield
            S.add("dve", lambda e: e.tensor_tensor(
                out=og_gT[:, :, bs], in0=PA[:, :].rearrange("p (h t) -> p h t", h=4), in1=u_sb[:], op=ALU.mult),
                reads=[pak, "u_sb"], writes=["og_gT"])
            yield

        def swa(s, b):
            first = (s % NSB_SEQ == 0 and b == 0)
            bs = slice(b * 128, (b + 1) * 128)
            pv = []
            dn = []
            for kv in range(2):
                ks_rows = slice(kv * 64, kv * 64 + 64)
                for pc in range(2):
                    if pc == 0 and first:
                        continue
                    bank = 2 + pc
                    if pc == 0:
                        if b == 0:
                            kl, kr = ks_carry[:, kv, :], "ks_carry"
                        else:
                            kl, kr = ksz[:, kv, (b - 1) * 128:b * 128], "ksT"
                    else:
                        kl, kr = ksz[:, kv, bs], "ksT"
                    S.add("pe", lambda e, bank=bank, kl=kl: e.matmul(
                        PS[bank][:, :], lhsT=kl, rhs=qsT[:, :, bs], start=True, stop=False),
                        reads=[kr, "qsT"], writes=["ps%d" % bank])
                    S.add("pe", lambda e, bank=bank, kv=kv, pc=pc: e.matmul(
                        PS[bank][:, :], lhsT=ident[:], rhs=mb[:, kv * 2 + pc, :], start=False, stop=True),
                        reads=["ident", "mb"], writes=["ps%d" % bank])
                    yield
                    pk = "p.%d" % (kv * 2 + pc)
                    S.add("act", lambda e, bank=bank, kv=kv, pc=pc: e.activation(
                        out=p_sb[:, kv * 2 + pc, :], in_=PS[bank][:, :], func=AF.Exp),
                        reads=["ps%d" % bank], writes=[pk])
                    yield
                    if pc == 0:
                        if b == 0:
                            vl, vr = vs_carry[:, kv * 128:(kv + 1) * 128], "vs_carry"
                        else:
                            vl, vr = vs_pad[:, b - 1, kv * 128:(kv + 1) * 128], "vs_pad.%d" % (b - 1)
                    else:
                        vl, vr = vs_pad[:, b, kv * 128:(kv + 1) * 128], "vs_pad.%d" % b
                    pv.append((vl, p_sb[:, kv * 2 + pc, :], [vr, pk]))
                    dn.append((Emat[:, kv, :], p_sb[:, kv * 2 + pc, :], ["Emat", pk]))
            dn.append((selT[32:36, 0:128], smallT[32:36, :], ["sel", "sel2", "sink_hi", "sink_lo"]))
            for i, (l, r, rd) in enumerate(pv):
                S.add("pe", lambda e, l=l, r=r, i=i: e.matmul(
                    PS[4][:, :], lhsT=l, rhs=r, start=(i == 0), stop=(i == len(pv) - 1)),
                    reads=rd, writes=["ps4"])
            yield
            for i, (l, r, rd) in enumerate(dn):
                S.add("pe", lambda e, l=l, r=r, i=i: e.matmul(
                    PS[5][:, :], lhsT=l, rhs=r, start=(i == 0), stop=(i == len(dn) - 1)),
                    reads=rd, writes=["ps5"])
            yield
            S.add("act", lambda e: e.activation(out=rden[:], in_=PS[5][:, :], func=AF.Ln), reads=["ps5"], writes=["rden"])
            yield
            S.add("act", lambda e: e.activation(out=rden[:], in_=rden[:], func=AF.Exp, scale=-1.0),
                  reads=["rden"], writes=["rden"])
            yield
            S.add("pool", lambda e: e.tensor_tensor(
                out=t_sb[:], in0=rden[:].rearrange("p (g t) -> p g t", g=4), in1=szs[:, :, bs], op=ALU.mult),
                reads=["rden", "szs"], writes=["t_sb"])
            yield
            S.add("dve", lambda e: e.tensor_tensor(
                out=os_gT[:, :, bs], in0=PS[4][:, :].rearrange("p (g t) -> p g t", g=4), in1=t_sb[:], op=ALU.mult),
                reads=["ps4", "t_sb"], writes=["os_gT"])
            yield
            if b == 3:
                S.add("pool", lambda e: e.tensor_copy(out=ks_carry[:], in_=ksz[:, :, 384:512]),
                      reads=["ksT"], writes=["ks_carry"])
                S.add("pool", lambda e: e.tensor_copy(out=vs_carry[:], in_=vs_pad[:, 3, :]),
                      reads=["vs_pad.3"], writes=["vs_carry"])
                yield

        def phase3(s):
            for j in range(8):
                if j % 2 == 0:
                    bga, bgb, bys, byg = 2, 3, 0, 7
                else:
                    bga, bgb, bys, byg = 4, 5, 1, 6
                kga, kgb, kys, kyg = pskeys(bga), pskeys(bgb), pskeys(bys), pskeys(byg)
                ca = C_GATE + j * 128
                cb = C_GATE + 1024 + j * 128
                for kc in range(8):
                    S.add("pe", lambda e, kc=kc, ca=ca, bga=bga: e.matmul(
                        PS[bga][:, :], lhsT=w_in_sb[:, kc, ca:ca + 128], rhs=xT[:, kc, :],
                        start=(kc == 0), stop=(kc == 7)),
                        reads=[wgrp(ca)] + XT_ALL, writes=kga)
                for kc in range(8):
                    S.add("pe", lambda e, kc=kc, cb=cb, bgb=bgb: e.matmul(
                        PS[bgb][:, :], lhsT=w_in_sb[:, kc, cb:cb + 128], rhs=xT[:, kc, :],
                        start=(kc == 0), stop=(kc == 7)),
                        reads=[wgrp(cb)] + XT_ALL, writes=kgb)
                for kc in range(4):
                    S.add("pe", lambda e, kc=kc, j=j, bys=bys: e.matmul(
                        PS[bys][:, :], lhsT=w_os_sb[:, kc, j * 128:(j + 1) * 128], rhs=os_gT[:, kc, :],
                        start=(kc == 0), stop=(kc == 3)),
                        reads=["w_os", "os_gT"], writes=kys)
                for kc in range(4):
                    S.add("pe", lambda e, kc=kc, j=j, byg=byg: e.matmul(
                        PS[byg][:, :], lhsT=w_og_sb[:, kc, j * 128:(j + 1) * 128], rhs=og_gT[:, kc, :],
                        start=(kc == 0), stop=(kc == 3)),
                        reads=["w_og", "og_gT"], writes=kyg)
                S.add("act", lambda e, j=j, bga=bga: e.activation(
                    out=ga_sb[:], in_=PS[bga][:, :], func=AF.Sigmoid, bias=bgate[:, j:j + 1]),
                    reads=kga + ["bgate"], writes=["ga_sb"])
                S.add("act", lambda e, j=j, bgb=bgb: e.activation(
                    out=gb_sb[:], in_=PS[bgb][:, :], func=AF.Sigmoid, bias=bgate[:, 8 + j:9 + j]),
                    reads=kgb + ["bgate"], writes=["gb_sb"])
                S.add("dve", lambda e, bys=bys: e.tensor_tensor(out=t2, in0=PS[bys][:, :], in1=gb_sb[:], op=ALU.mult),
                      reads=kys + ["gb_sb"], writes=["t_sb"])
                S.add("dve", lambda e, byg=byg: e.tensor_tensor(out=t1, in0=PS[byg][:, :], in1=ga_sb[:], op=ALU.mult),
                      reads=kyg + ["ga_sb"], writes=["u_sb"])
                S.add("pool", lambda e, j=j: e.tensor_tensor(out=mT[:, j, :], in0=t1, in1=t2, op=ALU.add),
                      reads=["u_sb", "t_sb"], writes=["mT"])

        def phase4(s, b):
            gb = s * 4 + b
            slot = gb % 2
            xk = "xr%d" % slot
            X = xr[slot]
            dma("sp", "xrl%d" % slot, X[:], x_d[gb * 128:(gb + 1) * 128, :], writes=[xk])
            yield
            for half in range(2):
                hsl = slice(half * 512, (half + 1) * 512)
                pb = 4 + half
                for kc in range(8):
                    S.add("pe", lambda e, kc=kc, hsl=hsl, pb=pb: e.matmul(
                        PS[pb][:, :], lhsT=mT[:, kc, b * 128:(b + 1) * 128], rhs=w_out_sb[:, kc, hsl],
                        start=(kc == 0), stop=(kc == 7)),
                        reads=["mT", "w_out"], writes=["ps%d" % pb])
                yield
                S.add("dve", lambda e, hsl=hsl, pb=pb: e.scalar_tensor_tensor(
                    out=X[:, hsl], in0=X[:, hsl], scalar=ALPHA, in1=PS[pb][:, :], op0=ALU.mult, op1=ALU.add),
                    reads=["ps%d" % pb, xk], writes=[xk])
                yield
            S.add("act", lambda e: e.activation(out=X[:], in_=X[:], func=AF.Identity, accum_out=st12[:, 0:1]),
                  reads=[xk], writes=[xk, "st1"])
            S.add("act", lambda e: e.activation(out=junk[:], in_=X[:], func=AF.Square, accum_out=st12[:, 1:2]),
                  reads=[xk], writes=["rs", "st2"])
            yield
            S.add("dve", lambda e: e.tensor_scalar(out=ms12[:], in0=st12[:], scalar1=1.0 / D, scalar2=None, op0=ALU.mult),
                  reads=["st1", "st2"], writes=["ms12"])
            S.add("dve", lambda e: e.scalar_tensor_tensor(
                out=nvar[:], in0=ms12[:, 0:1], scalar=ms12[:, 0:1], in1=ms12[:, 1:2], op0=ALU.mult, op1=ALU.subtract),
                reads=["ms12"], writes=["nvar"])
            yield
            S.add("dve", lambda e: e.tensor_scalar(out=vpe[:], in0=nvar[:], scalar1=-1.0, scalar2=LN_EPS,
                                                   op0=ALU.mult, op1=ALU.add),
                  reads=["nvar"], writes=["vpe"])
            S.add("pool", lambda e: e.tensor_tensor(out=rstd[:], in0=vpe[:], in1=mhalf[:], op=ALU.pow),
                  reads=["vpe", "mhalf"], writes=["rstd"])
            yield
            S.add("dve", lambda e: e.scalar_tensor_tensor(
                out=nbias[:], in0=ms12[:, 0:1], scalar=-1.0, in1=rstd[:], op0=ALU.mult, op1=ALU.mult),
                reads=["ms12", "rstd"], writes=["nbias"])
            yield
            S.add("act", lambda e: e.activation(out=X[:], in_=X[:], func=AF.Identity, scale=rstd[:, 0:1], bias=nbias[:, 0:1]),
                  reads=[xk, "rstd", "nbias"], writes=[xk])
            yield
            S.add("pool", lambda e: e.tensor_tensor(out=X[:], in0=X[:], in1=lng[:], op=ALU.mult),
                  reads=[xk, "lng"], writes=[xk])
            yield
            S.add("pool", lambda e: e.tensor_tensor(out=X[:], in0=X[:], in1=lnb[:], op=ALU.add),
                  reads=[xk, "lnb"], writes=[xk])
            yield
            dma("sp", "st%d" % slot, out_d[gb * 128:(gb + 1) * 128, :], X[:], reads=[xk])
            yield

        def staggered(groups):
            state = []
            for grp in groups:
                gens, lag = grp[0], grp[1]
                init = list(grp[2]) if len(grp) > 2 else [0] * len(gens)
                state.append({"gens": list(gens), "lag": lag, "steps": init, "done": [False] * len(gens)})
            while True:
                progressed = False
                for st in state:
                    for k, g in enumerate(st["gens"]):
                        if st["done"][k]:
                            continue
                        if k > 0 and not st["done"][k - 1] and st["steps"][k - 1] < st["lag"]:
                            continue
                        try:
                            next(g)
                            st["steps"][k] += 1
                        except StopIteration:
                            st["done"][k] = True
                        progressed = True
                if not progressed:
                    break

        def timed(items):
            st = [[g, f, n0, False] for g, f, n0 in items]
            while True:
                best = None
                for k, (g, f, n, done) in enumerate(st):
                    if done:
                        continue
                    t = f(n + 1)
                    if best is None or t < best[0]:
                        best = (t, k)
                if best is None:
                    break
                k = best[1]
                try:
                    next(st[k][0])
                    st[k][2] += 1
                except StopIteration:
                    st[k][3] = True

        def delayed_prefix(gen, delay, nsteps):
            for _ in range(delay):
                yield
            for _ in range(nsteps):
                try:
                    next(gen)
                except StopIteration:
                    return
                yield

        HOIST = int(os.environ.get("KERNEL_HOIST", "8"))
        GL_LAG = int(os.environ.get("KERNEL_GL_LAG", "7"))
        SW_LAG = int(os.environ.get("KERNEL_SW_LAG", "10"))
        P4_LAG = int(os.environ.get("KERNEL_P4_LAG", "7"))
        FRONT_EARLY = int(os.environ.get("KERNEL_FRONT_EARLY", "1"))
        SCHED = int(os.environ.get("KERNEL_SCHED", "2"))
        SP = float(os.environ.get("KERNEL_SP", "14"))
        SF0 = float(os.environ.get("KERNEL_SF0", "6"))
        SFS = float(os.environ.get("KERNEL_SFS", "1.2"))
        SB0 = float(os.environ.get("KERNEL_SB0", "1.5"))
        SBS = float(os.environ.get("KERNEL_SBS", "1.0"))
        SSS = float(os.environ.get("KERNEL_SSS", "1.0"))
        prev = None
        for idx, s in enumerate(sb_list):
            nxt = sb_list[idx + 1] if idx + 1 < len(sb_list) else None
            phase0(s, preloaded=(0, 1, 2, 3), nxt=nxt)
            g_gla = [gla(s, b) for b in range(4)]
            g_swa = [swa(s, b) for b in range(4)]
            p4s = [phase4(prev, b) for b in range(4)] if prev is not None else []
            groups = [([phase1(s)], 0)]
            if p4s:
                groups.append((p4s, P4_LAG))
            if HOIST > 0:
                groups.append(([delayed_prefix(g_gla[0], 11, HOIST)], 0))
            staggered(groups)
            if SCHED == 0:
                def t_gla(b, i):
                    return b * GL_LAG + i - (FRONT_EARLY if (i <= 8 and b > 0) else 0) + 0.5
                def t_swa(b, i):
                    return b * SW_LAG + i + 0.25
            elif SCHED == 2:
                FRONT_T = [3.2, 3.3, 3.4, 8.2, 8.3, 8.4, 11.8, 11.9]
                BACK_T = [1.5, 2.5, 7.5, 7.6, 8.5, 8.6, 9.5, 11.5, 12.5, 13.5, 14.5, 15.5]
                def t_gla(b, i):
                    if i <= 8:
                        return 14.0 * (b - 1) + FRONT_T[i - 1]
                    return 14.0 * b + (BACK_T[i - 9] if i <= 20 else BACK_T[-1] + (i - 20))
                def t_swa(b, i):
                    return 14.0 * b + i
            else:
                def t_gla(b, i):
                    if i <= 8:
                        return SP * (b - 1) + SF0 + (i - 1) * SFS
                    return SP * b + SB0 + (i - 9) * SBS
                def t_swa(b, i):
                    return SP * b + i * SSS
            timed([(g_gla[b], (lambda i, b=b: t_gla(b, i)), (HOIST if b == 0 else 0)) for b in range(4)] +
                  [(g_swa[b], (lambda i, b=b: t_swa(b, i)), 0) for b in range(4)])
            phase3(s)
            prev = s
        staggered([([phase4(prev, b) for b in range(4)], P4_LAG)])

        S.finalize()
        sems = {}
        for key in S.sem_keys:
            sems[key] = es.enter_context(nc.semaphore("s_%s_%s" % key))
        final_waits = [k for k in S.sem_keys if k[0] == "dma" and k[1].startswith("st")]
        with nc.Block() as block:
            S.emit(nc, block, sems, final_waits)
    return nc


def _consts():
    f = np.float32
    ident = np.eye(128, dtype=f)
    m = np.arange(128)
    tri = (m[:, None] <= m[None, :]).astype(f)
    mask = np.tile(tri, (1, 4))
    mbm = np.zeros((128, 4, 4, 128), dtype=f)
    s_ = m[:, None]
    q_ = m[None, :]
    NEG = -30000.0
    for kv in range(2):
        for g in range(4):
            slope = 2.0 ** (-(kv * 4 + g + 1))
            dist_prev = (q_ + 128 - s_).astype(f)
            dist_cur = (q_ - s_).astype(f)
            mbm[:, kv * 2 + 0, g, :] = np.where(s_ > q_, -slope * dist_prev, NEG)
            mbm[:, kv * 2 + 1, g, :] = np.where(s_ <= q_, -slope * dist_cur, NEG)
    E = np.zeros((128, 2, 128), dtype=f)
    E[:, 0, 0:64] = 1.0
    E[:, 1, 64:128] = 1.0
    sel = np.zeros((2, 128), dtype=f)
    sel[0, 0:64] = 1.0
    sel[1, 64:128] = 1.0
    gk = np.zeros((16, 512), dtype=f)
    gk[0, :] = 1.0
    return dict(c_ident=ident, c_mask=np.ascontiguousarray(mask),
                c_mb=np.ascontiguousarray(mbm.reshape(128, 2048)),
                c_E=np.ascontiguousarray(E.reshape(128, 256)), c_sel=sel, c_gk=gk)


def _pair_perm():
    idx = []
    for j in range(4):
        idx.extend(range(j * 64, j * 64 + 64))
        idx.extend(range((4 + j) * 64, (4 + j) * 64 + 64))
    return np.array(idx)


_NC_CACHE = {}


def kernel(x, w_in, w_gk2, b_gk, gla_norm_g, w_o_gla, sinks, w_o_swa, b_gate, w_out, ln_g, ln_b):
    f = np.float32
    nsb = int(os.environ.get("KERNEL_NSB", "16"))
    sb_list = list(range(nsb))
    x = np.asarray(x, dtype=f)
    perm = _pair_perm()
    w_in_p = np.array(w_in, dtype=f, copy=True)
    w_in_p[:, C_QS:C_QS + 512] = np.asarray(w_in)[:, C_QS + perm]
    w_in_p[:, C_ZS:C_ZS + 512] = np.asarray(w_in)[:, C_ZS + perm]
    w_os_p = np.ascontiguousarray(np.asarray(w_o_swa, dtype=f)[perm, :])
    wgk_aug = np.zeros((32, 256), dtype=f)
    wgk_aug[0:16] = np.asarray(w_gk2, dtype=f)
    wgk_aug[16] = np.asarray(b_gk, dtype=f)
    shared = dict(
        w_in=np.ascontiguousarray(w_in_p), w_o_gla=np.ascontiguousarray(np.asarray(w_o_gla, dtype=f)),
        w_o_swa=w_os_p, w_out=np.ascontiguousarray(np.asarray(w_out, dtype=f)), wgk_aug=wgk_aug,
        gnorm=np.ascontiguousarray(np.asarray(gla_norm_g, dtype=f).reshape(128, 1)),
        sinks_rep=np.ascontiguousarray(np.repeat(np.asarray(sinks, dtype=f).reshape(2, 4), 128, axis=1)),
        bgate=np.ascontiguousarray(np.asarray(b_gate, dtype=f).reshape(16, 128).T),
        lng_bc=np.ascontiguousarray(np.broadcast_to(np.asarray(ln_g, dtype=f)[None, :], (128, D))),
        lnb_bc=np.ascontiguousarray(np.broadcast_to(np.asarray(ln_b, dtype=f)[None, :], (128, D))),
    )
    shared.update(_consts())
    key = tuple(sb_list)
    if key not in _NC_CACHE:
        _NC_CACHE[key] = build_nc(sb_list)
    nc = _NC_CACHE[key]
    xs = x.reshape(N_CORES, NTOK, D)
    in_maps = []
    for c in range(N_CORES):
        m = dict(shared)
        m["x"] = np.ascontiguousarray(xs[c])
        in_maps.append(m)
    res = run_bass_kernel_spmd(nc, in_maps, core_ids=list(range(N_CORES)))
    outs = [np.asarray(r["out"], dtype=f).reshape(SEQ_PER_CORE, SEQ, D) for r in res.results]
    return np.concatenate(outs, axis=0).astype(f)
```

```python
import os
from contextlib import ExitStack

import numpy as np
import concourse.bass as bass
import concourse.mybir as mybir
from concourse.bass_utils import run_bass_kernel_spmd

F32 = mybir.dt.float32
BF16 = mybir.dt.bfloat16
AF = mybir.ActivationFunctionType
ALU = mybir.AluOpType

N_CORES = 8
D = 1024
SEQ = 2048
SEQ_PER_CORE = 4
NTOK = SEQ * SEQ_PER_CORE
T = 512
NSB_SEQ = SEQ // T
D_IN = 4880
ALPHA = 2.0 ** 0.25
LN_EPS = 1e-5
RMS_EPS = 1e-6

C_QG, C_KG, C_VG, C_GK, C_ZG, C_QS, C_KS, C_VS, C_ZS, C_GATE = (
    0, 256, 512, 1024, 1040, 1552, 2064, 2192, 2320, 2832)
W_GROUPS = [(0, 512), (512, 1040), (1040, 1552), (1552, 2064), (2064, 2320),
            (2320, 2832), (2832, 3344), (3344, 3856), (3856, 4368), (4368, 4880)]


def pskeys(bank):
    return ["ps%d" % bank]


def wgrp(c):
    for i, (a, b) in enumerate(W_GROUPS):
        if a <= c < b:
            return "w_in.%d" % i
    raise ValueError(c)


class _Op:
    __slots__ = ("eng", "fn", "raw", "oth", "dma", "need_inc", "done", "waits")

    def __init__(self, eng, fn, raw, oth, dma):
        self.eng, self.fn, self.raw, self.oth, self.dma = eng, fn, raw, oth, dma
        self.need_inc = False
        self.done = None
        self.waits = []


class Sched:
    ENGS = ("pe", "act", "dve", "pool", "sp")

    def __init__(self):
        self.ops = []
        self.last_w = {}
        self.readers = {}

    def add(self, eng, fn, reads=(), writes=(), dma=None):
        i = len(self.ops)
        raw, oth = set(), set()
        for r in reads:
            w = self.last_w.get(r)
            if w is not None:
                raw.add(w)
        for k in writes:
            w = self.last_w.get(k)
            if w is not None:
                raw.add(w)
            for rd in self.readers.get(k, ()):
                oth.add(rd)
        for r in reads:
            self.readers.setdefault(r, []).append(i)
        for k in writes:
            self.last_w[k] = i
            self.readers[k] = []
        self.ops.append(_Op(eng, fn, raw, oth - raw, dma))
        return i

    def finalize(self):
        ops = self.ops
        for op in ops:
            deps = set()
            for d in op.raw:
                p = ops[d]
                deps.add(d)
            for d in op.oth:
                p = ops[d]
                if p.eng == op.eng and p.dma is None:
                    continue
                deps.add(d)
            if op.eng == "pe":
                deps = {d for d in deps if not (ops[d].eng == "pe" and ops[d].dma is None)}
            op.raw = deps
            for d in deps:
                ops[d].need_inc = True
        cnt = {}
        self.sem_keys = []
        for op in ops:
            if op.dma is not None:
                key = ("dma", op.dma)
                cnt[key] = cnt.get(key, 0) + 16
                op.done = (key, cnt[key])
            elif op.need_inc:
                key = ("eng", op.eng)
                cnt[key] = cnt.get(key, 0) + 1
                op.done = (key, cnt[key])
            else:
                continue
            if key not in self.sem_keys:
                self.sem_keys.append(key)
        self.final_cnt = cnt
        seen = {e: {} for e in self.ENGS}
        for op in ops:
            w = {}
            for d in op.raw:
                key, c = ops[d].done
                if w.get(key, 0) < c:
                    w[key] = c
            sw = seen[op.eng]
            op.waits = []
            for key, c in w.items():
                if sw.get(key, 0) < c:
                    op.waits.append((key, c))
                    sw[key] = c

    def emit(self, nc, block, sems, final_waits):
        reg = {"pe": block.tensor, "act": block.scalar, "dve": block.vector,
               "pool": block.gpsimd, "sp": block.sync}
        for eng in self.ENGS:
            ops_e = [op for op in self.ops if op.eng == eng]

            def body(e, ops_e=ops_e, eng=eng):
                for op in ops_e:
                    for key, c in op.waits:
                        e.wait_ge(sems[key], c)
                    ins = op.fn(e)
                    if op.dma is not None:
                        ins.then_inc(sems[("dma", op.dma)], 16)
                    elif op.need_inc:
                        ins.then_inc(sems[("eng", eng)], 1)
                if eng == "sp":
                    for key in final_waits:
                        e.wait_ge(sems[key], self.final_cnt[key])

            reg[eng](body)


def _interleave(gens):
    gens = list(gens)
    while gens:
        for g in list(gens):
            try:
                next(g)
            except StopIteration:
                gens.remove(g)


def build_nc(sb_list):
    nc = bass.Bass("TRN2", target_bir_lowering=False)

    def din(name, shape, dt=F32):
        return nc.dram_tensor(name, list(shape), dt, kind="ExternalInput").ap()

    x_d = din("x", [NTOK, D])
    w_in_d = din("w_in", [D, D_IN])
    w_og_d = din("w_o_gla", [512, D])
    w_os_d = din("w_o_swa", [512, D])
    w_out_d = din("w_out", [D, D])
    wgk_d = din("wgk_aug", [32, 256])
    gnorm_d = din("gnorm", [128, 1])
    sinks_d = din("sinks_rep", [2, 512])
    bgate_d = din("bgate", [128, 16])
    lng_d = din("lng_bc", [128, D])
    lnb_d = din("lnb_bc", [128, D])
    c_ident_d = din("c_ident", [128, 128])
    c_mask_d = din("c_mask", [128, 512])
    c_mb_d = din("c_mb", [128, 2048])
    c_E_d = din("c_E", [128, 256])
    c_sel_d = din("c_sel", [2, 128])
    c_gk_d = din("c_gk", [16, 512])
    out_d = nc.dram_tensor("out", [NTOK, D], F32, kind="ExternalOutput").ap()

    S = Sched()
    es = ExitStack()

    def sb(name, shape, dt):
        return es.enter_context(nc.sbuf_tensor(name, list(shape), dt))

    with es:
        w_in_sb = sb("w_in_sb", [128, 8, D_IN], BF16)
        w_og_sb = sb("w_og_sb", [128, 4, D], BF16)
        w_os_sb = sb("w_os_sb", [128, 4, D], BF16)
        w_out_sb = sb("w_out_sb", [128, 8, D], BF16)
        ident = sb("ident", [128, 128], BF16)
        tri = sb("tri", [128, 128], F32)
        mb = sb("mb", [128, 4, 512], BF16)
        ones = sb("ones", [128, 128], BF16)
        Emat = sb("Emat", [128, 2, 128], BF16)
        wgk_t = sb("wgk", [36, 256], BF16)
        wgk = wgk_t[0:32, :]
        selT = wgk_t
        gnorm = sb("gnorm_sb", [128, 1], F32)
        bgate = sb("bgate_sb", [128, 16], F32)
        lng = sb("lng", [128, D], F32)
        lnb = sb("lnb", [128, D], F32)

        xbf = [sb("xbf%d" % i, [128, D], BF16) for i in range(4)]
        xT = sb("xT", [128, 8, T], BF16)
        qgT = sb("qgT", [128, 2, T], BF16)
        kgT = sb("kgT", [128, 2, T], BF16)
        szg = sb("szg", [128, 4, T], BF16)
        qsT = sb("qsT", [128, 4, T], BF16)
        ksz = sb("ksz", [128, 2, T], BF16)
        szs = sb("szs", [128, 4, T], BF16)
        smallT = sb("smallT", [128, T], BF16)
        gkT = smallT[0:32, :]
        sink_hi = smallT[32:34, :]
        sink_lo = smallT[34:36, :]
        sel_hi = selT[32:34, 0:128]
        sel_lo = selT[34:36, 0:128]
        vg_tok = sb("vg_tok", [128, 4, 512], BF16)
        vs_pad = sb("vs_pad", [128, 4, 256], BF16)
        ks_carry = sb("ks_carry", [128, 2, 128], BF16)
        vs_carry = sb("vs_carry", [128, 256], BF16)

        sp_sb = sb("sp_sb", [128, 256], F32)
        eGi = sb("eGi", [128, 2, 128], F32)
        dec = [sb("dec%d" % i, [128, 2, 1], F32) for i in range(4)]
        decm = sb("decm", [128, 2, 2, 1], F32)
        halfmask = sb("halfmask", [128, 2, 2, 1], F32)
        mhalf = sb("mhalf", [128, 1], F32)
        vpe = sb("vpe", [128, 1], F32)
        q_dec = sb("q_dec", [128, 2, 128], BF16)
        kinv_tok = sb("kinv_tok", [128, 256], BF16)
        k_inv = kinv_tok[:].rearrange("p (c t) -> p c t", c=2)
        AT_m = sb("AT_m", [128, 512], BF16)
        Rst = sb("Rst", [128, 2, 128], F32)
        Sz = sb("Sz", [128, 2, 2, 128], BF16)
        kz = sb("kz", [128, 2, 2, 128], BF16)
        rs = sb("rs", [128, 512], F32)
        u_sb = sb("u_sb", [128, 4, 128], BF16)
        p_sb = sb("p_sb", [128, 4, 512], BF16)
        rden = sb("rden", [128, 512], F32)
        t_sb = sb("t_sb", [128, 4, 128], BF16)
        og_gT = sb("og_gT", [128, 4, T], BF16)
        os_gT = sb("os_gT", [128, 4, T], BF16)

        ga_sb = sb("ga_sb", [128, T], BF16)
        gb_sb = sb("gb_sb", [128, T], BF16)
        mT = sb("mT", [128, 8, T], BF16)

        xr = [sb("xr%d" % i, [128, D], F32) for i in range(2)]
        st12 = sb("st12", [128, 2], F32)
        ms12 = sb("ms12", [128, 2], F32)
        nvar = sb("nvar", [128, 1], F32)
        rstd = sb("rstd", [128, 1], F32)
        nbias = sb("nbias", [128, 1], F32)

        eG = sp_sb[:].rearrange("p (c t) -> p c t", c=2)
        t1 = u_sb[:].rearrange("p h t -> p (h t)")
        t2 = t_sb[:].rearrange("p h t -> p (h t)")
        PS = [es.enter_context(nc.psum_tensor("ps%d" % i, [128, 512], F32)) for i in range(8)]
        PSb7 = PS[7].bitcast(BF16)
        PSb0 = PS[0].bitcast(BF16)
        junk = rs.bitcast(BF16)

        def dma(eng, key, out, in_, reads=(), writes=()):
            S.add(eng, lambda e, o=out, i=in_: e.dma_start(out=o, in_=i),
                  reads=reads, writes=writes, dma=key)

        w_in_v = w_in_d.rearrange("(kc p) n -> p kc n", p=128)
        dma("pool", "c_ident", ident[:], c_ident_d, writes=["ident"])
        dma("pool", "c_gk", smallT[16:32, :], c_gk_d, writes=["gkT.c"])
        def wload(gi):
            a, b = W_GROUPS[gi]
            dma("pool", "w_in.%d" % gi, w_in_sb[:, :, a:b], w_in_v[:, :, a:b], writes=["w_in.%d" % gi])

        first_s = sb_list[0]
        for b in range(4):
            gb0 = first_s * 4 + b
            dma("pool", "xbf%d" % b, xbf[b][:], x_d[gb0 * 128:(gb0 + 1) * 128, :], writes=["xbf%d" % b])
        wload(0)
        wload(1)
        dma("pool", "c_wgk", wgk, wgk_d, writes=["wgk"])
        dma("sp", "c_tri", tri[:], c_mask_d[:, 0:128], writes=["tri"])
        dma("sp", "c_gnorm", gnorm[:], gnorm_d, writes=["gnorm"])
        sink_f = rden[0:2, :]
        sink_hi32 = rs[0:2, :]
        dma("sp", "c_sinks", sink_f, sinks_d, writes=["rden"])
        dma("sp", "c_bgate", bgate[:], bgate_d, writes=["bgate"])
        dma("sp", "c_lng", lng[:], lng_d, writes=["lng"])
        dma("sp", "c_lnb", lnb[:], lnb_d, writes=["lnb"])
        for gi in (3, 4, 2, 5):
            wload(gi)
        dma("pool", "c_mb", mb[:].rearrange("p a b -> p (a b)"), c_mb_d, writes=["mb"])
        dma("pool", "c_E", Emat[:].rearrange("p a b -> p (a b)"), c_E_d, writes=["Emat"])
        dma("pool", "c_sel", sel_hi, c_sel_d, writes=["sel"])
        dma("pool", "c_sel2", sel_lo, c_sel_d, writes=["sel2"])
        for gi in (6, 8):
            wload(gi)
        dma("pool", "w_os", w_os_sb[:], w_os_d.rearrange("(kc p) n -> p kc n", p=128), writes=["w_os"])
        dma("pool", "w_og", w_og_sb[:], w_og_d.rearrange("(kc p) n -> p kc n", p=128), writes=["w_og"])
        for gi in (7, 9):
            wload(gi)
        dma("pool", "w_out", w_out_sb[:], w_out_d.rearrange("(kc p) n -> p kc n", p=128), writes=["w_out"])

        S.add("dve", lambda e: e.memset(ones[:], 1.0), writes=["ones"])
        S.add("dve", lambda e: e.memset(vs_pad[:], 0.0), writes=["vs_pad.%d" % b for b in range(4)])
        S.add("dve", lambda e: e.memset(vs_carry[:], 0.0), writes=["vs_carry"])
        S.add("dve", lambda e: e.memset(ksz[:], 0.0), writes=["ksT"])
        S.add("dve", lambda e: e.memset(ks_carry[:], 0.0), writes=["ks_carry"])
        S.add("dve", lambda e: e.memset(Sz[:], 0.0), writes=["Sbf"])
        S.add("dve", lambda e: e.memset(kz[:], 0.0), writes=["kz"])
        S.add("dve", lambda e: e.memset(mhalf[:], -0.5), writes=["mhalf"])
        S.add("dve", lambda e: e.memset(halfmask[:], 0.0), writes=["halfmask"])
        S.add("dve", lambda e: e.memset(halfmask[0:64, :, 0, :], 1.0), writes=["halfmask"])
        S.add("dve", lambda e: e.memset(halfmask[64:128, :, 1, :], 1.0), writes=["halfmask"])
        S.add("act", lambda e: e.activation(out=sink_f, in_=sink_f, func=AF.Exp),
              reads=["rden"], writes=["rden"])
        sink_t = p_sb[0:2, 0:2, :]
        S.add("dve", lambda e: e.tensor_copy(out=sink_t[:, 0, :], in_=sink_f), reads=["rden"], writes=["p.0"])
        S.add("dve", lambda e: e.tensor_copy(out=sink_hi32, in_=sink_t[:, 0, :]), reads=["p.0"], writes=["rs"])
        S.add("dve", lambda e: e.tensor_tensor(out=sink_t[:, 1, :], in0=sink_f, in1=sink_hi32, op=ALU.subtract),
              reads=["rden", "rs"], writes=["p.1"])
        dma("sp", "c_sh", sink_hi, sink_t[:, 0, :], reads=["p.0"], writes=["sink_hi"])
        dma("sp", "c_sl", sink_lo, sink_t[:, 1, :], reads=["p.1"], writes=["sink_lo"])

        W_ALL = ["w_in.%d" % i for i in range(10)]
        XT_ALL = ["xT.%d" % b for b in range(4)]

        def emit_xload(s, b):
            gb = s * 4 + b
            slot = b
            dma("pool", "xbf%d" % slot, xbf[slot][:], x_d[gb * 128:(gb + 1) * 128, :],
                writes=["xbf%d" % slot])

        def phase0(s, preloaded, nxt=None):
            for b in range(4):
                if b not in preloaded:
                    emit_xload(s, b)
                slot = b
                pbk, pkey = ((PSb7, "ps7"), (PSb0, "ps0"))[b % 2]
                for kc in range(8):
                    S.add("pe", lambda e, kc=kc, slot=slot, pbk=pbk: e.transpose(
                        out=pbk[:, kc * 128:(kc + 1) * 128], in_=xbf[slot][:, kc * 128:(kc + 1) * 128],
                        identity=ident[:]),
                        reads=["xbf%d" % slot, "ident"], writes=[pkey])
                S.add(("dve", "act")[b % 2], lambda e, b=b, pbk=pbk: (
                    e.tensor_copy(out=xT[:, :, b * 128:(b + 1) * 128],
                                  in_=pbk[:].rearrange("p (k t) -> p k t", k=8)) if b % 2 == 0 else
                    e.activation(out=xT[:, :, b * 128:(b + 1) * 128],
                                 in_=pbk[:].rearrange("p (k t) -> p k t", k=8), func=AF.Copy)),
                    reads=[pkey], writes=["xT.%d" % b])
                if nxt is not None:
                    emit_xload(nxt, b)

        def phase1(s):
            chunks = []
            for c in range(2):
                chunks.append(("qg", c, C_QG + c * 128, 128))
            for c in range(2):
                chunks.append(("kg", c, C_KG + c * 128, 128))
            chunks.append(("gk", 0, C_GK, 128))
            for c in range(4):
                chunks.append(("qs", c, C_QS + c * 128, 128))
            chunks.append(("ks", 0, C_KS, 128))
            for c in range(4):
                chunks.append(("zg", c, C_ZG + c * 128, 128))
            for c in range(4):
                chunks.append(("zs", c, C_ZS + c * 128, 128))
            for i, (kind, c, c0, M) in enumerate(chunks):
                if i > 0:
                    yield
                bank = (1, 6, 2, 3)[i % 4]
                for kc in range(8):
                    if kc == 4:
                        yield
                    S.add("pe", lambda e, kc=kc, bank=bank, c0=c0, M=M: e.matmul(
                        PS[bank][0:M, :], lhsT=w_in_sb[:, kc, c0:c0 + M], rhs=xT[:, kc, :],
                        start=(kc == 0), stop=(kc == 7)),
                        reads=sorted({wgrp(c0), wgrp(c0 + M - 1)}) + XT_ALL, writes=pskeys(bank))
                src = PS[bank]
                rd = pskeys(bank)
                if kind == "qg":
                    S.add("dve", lambda e, c=c, src=src: e.tensor_scalar(
                        out=qgT[:, c, :], in0=src[:], scalar1=0.125, scalar2=None, op0=ALU.mult),
                        reads=rd, writes=["qgT"])
                elif kind == "kg":
                    S.add("dve", lambda e, c=c, src=src: e.tensor_copy(out=kgT[:, c, :], in_=src[:]),
                          reads=rd, writes=["kgT"])
                elif kind == "gk":
                    S.add("dve", lambda e, src=src: e.tensor_copy(out=smallT[0:16, :], in_=src[0:16, :]),
                          reads=rd, writes=["gkT"])
                elif kind == "qs":
                    S.add("act", lambda e, c=c, src=src: e.activation(
                        out=qsT[:, c, :], in_=src[:], func=AF.Copy, scale=0.125),
                        reads=rd, writes=["qsT"])
                elif kind == "ks":
                    for kv in range(2):
                        S.add("dve", lambda e, src=src, kv=kv: e.tensor_copy(
                            out=ksz[kv * 64:(kv + 1) * 64, kv, :], in_=src[kv * 64:(kv + 1) * 64, :]),
                            reads=rd, writes=["ksT"])
                elif kind == "zg":
                    S.add("act", lambda e, c=c, src=src: e.activation(
                        out=szg[:, c, :], in_=src[:], func=AF.Silu),
                        reads=rd, writes=["szg"])
                elif kind == "zs":
                    S.add("act", lambda e, c=c, src=src: e.activation(
                        out=szs[:, c, :], in_=src[:], func=AF.Silu),
                        reads=rd, writes=["szs"])
            for b in range(4):
                yield
                for kc in range(8):
                    S.add("pe", lambda e, kc=kc, b=b: e.matmul(
                        PS[2][:, :], lhsT=xT[:, kc, b * 128:(b + 1) * 128], rhs=w_in_sb[:, kc, C_VG:C_VG + 512],
                        start=(kc == 0), stop=(kc == 7)),
                        reads=[wgrp(C_VG), "xT.%d" % b], writes=["ps2"])
                for kc in range(8):
                    S.add("pe", lambda e, kc=kc, b=b: e.matmul(
                        PS[3][:, 0:128], lhsT=xT[:, kc, b * 128:(b + 1) * 128], rhs=w_in_sb[:, kc, C_VS:C_VS + 128],
                        start=(kc == 0), stop=(kc == 7)),
                        reads=[wgrp(C_VS), "xT.%d" % b], writes=["ps3"])
                S.add("dve", lambda e, b=b: e.tensor_copy(out=vg_tok[:, b, :], in_=PS[2][:, :]),
                      reads=["ps2"], writes=["vg_tok.%d" % b])
                for kv in range(2):
                    S.add("act", lambda e, b=b, kv=kv: e.activation(
                        out=vs_pad[:, b, kv * 128 + kv * 64: kv * 128 + kv * 64 + 64],
                        in_=PS[3][:, kv * 64:(kv + 1) * 64], func=AF.Copy),
                        reads=["ps3"], writes=["vs_pad.%d" % b])

        def gla(s, b):
            first = (s % NSB_SEQ == 0 and b == 0)
            gb = s * 4 + b
            dcur, dprev = dec[gb % 4], dec[(gb + 3) % 4]
            dck, dpk = "dec%d" % (gb % 4), "dec%d" % ((gb + 3) % 4)
            bs = slice(b * 128, (b + 1) * 128)
            PA, pak = (PS[1], "ps1") if b % 2 == 0 else (PS[7], "ps7")
            S.add("pe", lambda e: e.matmul(PS[0][:, 0:256], lhsT=smallT[0:32, bs], rhs=wgk, start=True, stop=True),
                  reads=["gkT", "gkT.c", "wgk"], writes=["ps0"])
            yield
            S.add("act", lambda e: e.activation(out=sp_sb[:], in_=PS[0][:, 0:256], func=AF.Exp, scale=-1.0),
                  reads=["ps0"], writes=["sp_sb"])
            yield
            S.add("act", lambda e: e.activation(out=sp_sb[:], in_=sp_sb[:], func=AF.Ln, bias=1.0),
                  reads=["sp_sb"], writes=["sp_sb"])
            yield
            for c in range(2):
                S.add("pe", lambda e, c=c: e.matmul(
                    PS[0][:, 256 + c * 128:256 + (c + 1) * 128], lhsT=sp_sb[:, c * 128:(c + 1) * 128], rhs=tri[:],
                    start=True, stop=True),
                    reads=["sp_sb", "tri"], writes=["ps0"])
            yield
            GTv = PS[0][:, 256:512].rearrange("p (c t) -> p c t", c=2)
            S.add("act", lambda e: e.activation(out=eG, in_=GTv, func=AF.Exp, scale=-1.0 / 16.0),
                  reads=["ps0"], writes=["sp_sb"])
            S.add("act", lambda e: e.activation(out=eGi[:], in_=GTv, func=AF.Exp, scale=1.0 / 16.0),
                  reads=["ps0"], writes=["eGi"])
            S.add("act", lambda e: e.activation(out=dcur[:], in_=GTv[:, :, 127:128], func=AF.Exp, scale=-1.0 / 16.0),
                  reads=["ps0"], writes=[dck])
            yield
            S.add("dve", lambda e: e.tensor_tensor(out=k_inv, in0=kgT[:, :, bs], in1=eGi[:], op=ALU.mult),
                  reads=["kgT", "eGi"], writes=["kinv_tok"])
            S.add("dve", lambda e: e.tensor_tensor(out=q_dec[:], in0=qgT[:, :, bs], in1=eG, op=ALU.mult),
                  reads=["qgT", "sp_sb"], writes=["q_dec"])
            for hh in range(2):
                hs = slice(hh * 64, hh * 64 + 64)
                S.add("dve", lambda e, hh=hh, hs=hs: e.tensor_tensor(
                    out=kz[hs, :, hh, :], in0=kgT[hs, :, bs], in1=eGi[hs, :, :], op=ALU.mult),
                    reads=["kgT", "eGi"], writes=["kz"])
            yield
            for c in range(2):
                S.add("pe", lambda e, c=c: e.transpose(
                    out=PSb0[:, c * 128:(c + 1) * 128], in_=k_inv[:, c, :], identity=ident[:]),
                    reads=["kinv_tok", "ident"], writes=["ps0"])
            yield
            S.add("dve", lambda e: e.tensor_copy(out=kinv_tok[:], in_=PSb0[:, 0:256]),
                  reads=["ps0"], writes=["kinv_tok"])
            yield
            for h in range(4):
                c, hs = h // 2, slice((h % 2) * 64, (h % 2) * 64 + 64)
                S.add("pe", lambda e, h=h, c=c: e.matmul(
                    PA[:, h * 128:(h + 1) * 128], lhsT=kz[:, c, h % 2, :], rhs=q_dec[:, c, :],
                    start=True, stop=True),
                    reads=["kz", "q_dec"], writes=[pak])
            yield
            S.add("dve", lambda e: e.tensor_tensor(
                out=AT_m[:].rearrange("p (h t) -> p h t", h=4), in0=PA[:, :].rearrange("p (h t) -> p h t", h=4),
                in1=tri[:].unsqueeze(1).to_broadcast([128, 4, 128]), op=ALU.mult),
                reads=[pak, "tri"], writes=["AT_m"])
            yield
            for h in range(4):
                c, hs = h // 2, slice((h % 2) * 64, (h % 2) * 64 + 64)
                S.add("pe", lambda e, h=h: e.matmul(
                    PA[:, h * 128:(h + 1) * 128], lhsT=vg_tok[:, b, h * 128:(h + 1) * 128],
                    rhs=AT_m[:, h * 128:(h + 1) * 128], start=True, stop=first),
                    reads=["vg_tok.%d" % b, "AT_m"], writes=[pak])
                if not first:
                    S.add("pe", lambda e, h=h, c=c: e.matmul(
                        PA[:, h * 128:(h + 1) * 128], lhsT=Sz[:, c, h % 2, :], rhs=q_dec[:, c, :],
                        start=False, stop=True),
                        reads=["Sbf", "q_dec"], writes=[pak])
            yield
            for c in range(2):
                S.add("pe", lambda e, c=c: e.matmul(
                    PS[6][:, c * 256:(c + 1) * 256], lhsT=kinv_tok[:, c * 128:(c + 1) * 128],
                    rhs=vg_tok[:, b, c * 256:(c + 1) * 256], start=True, stop=True),
                    reads=["kinv_tok", "vg_tok.%d" % b], writes=["ps6"])
            yield
            for c in range(2):
                for hh in range(2):
                    hs = slice(hh * 64, hh * 64 + 64)
                    src = PS[6][hs, c * 256 + hh * 128: c * 256 + hh * 128 + 128]
                    if first:
                        S.add("dve", lambda e, c=c, hs=hs, src=src: e.tensor_copy(out=Rst[hs, c, :], in_=src),
                              reads=["ps6"], writes=["Rst"])
                    else:
                        S.add("dve", lambda e, c=c, hs=hs, src=src: e.scalar_tensor_tensor(
                            out=Rst[hs, c, :], in0=Rst[hs, c, :], scalar=dprev[hs, c, :], in1=src,
                            op0=ALU.mult, op1=ALU.add),
                            reads=["ps6", "Rst", dpk], writes=["Rst"])
            yield
            S.add("dve", lambda e: e.tensor_tensor(
                out=decm[:], in0=dcur[:].unsqueeze(2).to_broadcast([128, 2, 2, 1]), in1=halfmask[:], op=ALU.mult),
                reads=[dck, "halfmask"], writes=["decm"])
            S.add("dve", lambda e: e.tensor_tensor(
                out=Sz[:], in0=Rst[:].unsqueeze(2).to_broadcast([128, 2, 2, 128]),
                in1=decm[:].to_broadcast([128, 2, 2, 128]), op=ALU.mult),
                reads=["Rst", "decm"], writes=["Sbf"])
            yield
            S.add("act", lambda e: e.activation(out=AT_m[:], in_=PA[:, :], func=AF.Square),
                  reads=[pak], writes=["AT_m"])
            yield
            S.add("pe", lambda e: e.matmul(PS[6][:, :], lhsT=ones[:], rhs=AT_m[:], start=True, stop=True),
                  reads=["ones", "AT_m"], writes=["ps6"])
            yield
            S.add("act", lambda e: e.activation(out=rs[:], in_=PS[6][:, :], func=AF.Ln, scale=1.0 / 128.0, bias=RMS_EPS),
                  reads=["ps6"], writes=["rs"])
            yield
            S.add("act", lambda e: e.activation(out=rs[:], in_=rs[:], func=AF.Exp, scale=-0.5),
                  reads=["rs"], writes=["rs"])
            yield
            S.add("dve", lambda e: e.scalar_tensor_tensor(
                out=u_sb[:], in0=rs[:].rearrange("p (h t) -> p h t", h=4), scalar=gnorm[:, 0:1],
                in1=szg[:, :, bs], op0=ALU.mult, op1=ALU.mult),
                reads=["rs", "gnorm", "szg"], writes=["u_sb"])
            yield
            S.add("dve", lambda e: e.tensor_tensor(
                out=og_gT[:, :, bs], in0=PA[:, :].rearrange("p (h t) -> p h t", h=4), in1=u_sb[:], op=ALU.mult),
                reads=[pak, "u_sb"], writes=["og_gT"])
            yield

        def swa(s, b):
            first = (s % NSB_SEQ == 0 and b == 0)
            bs = slice(b * 128, (b + 1) * 128)
            pv = []
            dn = []
            for kv in range(2):
                ks_rows = slice(kv * 64, kv * 64 + 64)
                for pc in range(2):
                    if pc == 0 and first:
                        continue
                    bank = 2 + pc
                    if pc == 0:
                        if b == 0:
                            kl, kr = ks_carry[:, kv, :], "ks_carry"
                        else:
                            kl, kr = ksz[:, kv, (b - 1) * 128:b * 128], "ksT"
                    else:
                        kl, kr = ksz[:, kv, bs], "ksT"
                    S.add("pe", lambda e, bank=bank, kl=kl: e.matmul(
                        PS[bank][:, :], lhsT=kl, rhs=qsT[:, :, bs], start=True, stop=False),
                        reads=[kr, "qsT"], writes=["ps%d" % bank])
                    S.add("pe", lambda e, bank=bank, kv=kv, pc=pc: e.matmul(
                        PS[bank][:, :], lhsT=ident[:], rhs=mb[:, kv * 2 + pc, :], start=False, stop=True),
                        reads=["ident", "mb"], writes=["ps%d" % bank])
                    yield
                    pk = "p.%d" % (kv * 2 + pc)
                    S.add("act", lambda e, bank=bank, kv=kv, pc=pc: e.activation(
                        out=p_sb[:, kv * 2 + pc, :], in_=PS[bank][:, :], func=AF.Exp),
                        reads=["ps%d" % bank], writes=[pk])
                    yield
                    if pc == 0:
                        if b == 0:
                            vl, vr = vs_carry[:, kv * 128:(kv + 1) * 128], "vs_carry"
                        else:
                            vl, vr = vs_pad[:, b - 1, kv * 128:(kv + 1) * 128], "vs_pad.%d" % (b - 1)
                    else:
                        vl, vr = vs_pad[:, b, kv * 128:(kv + 1) * 128], "vs_pad.%d" % b
                    pv.append((vl, p_sb[:, kv * 2 + pc, :], [vr, pk]))
                    dn.append((Emat[:, kv, :], p_sb[:, kv * 2 + pc, :], ["Emat", pk]))
            dn.append((selT[32:36, 0:128], smallT[32:36, :], ["sel", "sel2", "sink_hi", "sink_lo"]))
            for i, (l, r, rd) in enumerate(pv):
                S.add("pe", lambda e, l=l, r=r, i=i: e.matmul(
                    PS[4][:, :], lhsT=l, rhs=r, start=(i == 0), stop=(i == len(pv) - 1)),
                    reads=rd, writes=["ps4"])
            yield
            for i, (l, r, rd) in enumerate(dn):
                S.add("pe", lambda e, l=l, r=r, i=i: e.matmul(
                    PS[5][:, :], lhsT=l, rhs=r, start=(i == 0), stop=(i == len(dn) - 1)),
                    reads=rd, writes=["ps5"])
            yield
            S.add("act", lambda e: e.activation(out=rden[:], in_=PS[5][:, :], func=AF.Ln), reads=["ps5"], writes=["rden"])
            yield
            S.add("act", lambda e: e.activation(out=rden[:], in_=rden[:], func=AF.Exp, scale=-1.0),
                  reads=["rden"], writes=["rden"])
            yield
            S.add("pool", lambda e: e.tensor_tensor(
                out=t_sb[:], in0=rden[:].rearrange("p (g t) -> p g t", g=4), in1=szs[:, :, bs], op=ALU.mult),
                reads=["rden", "szs"], writes=["t_sb"])
            yield
            S.add("dve", lambda e: e.tensor_tensor(
                out=os_gT[:, :, bs], in0=PS[4][:, :].rearrange("p (g t) -> p g t", g=4), in1=t_sb[:], op=ALU.mult),
                reads=["ps4", "t_sb"], writes=["os_gT"])
            yield
            if b == 3:
                S.add("pool", lambda e: e.tensor_copy(out=ks_carry[:], in_=ksz[:, :, 384:512]),
                      reads=["ksT"], writes=["ks_carry"])
                S.add("pool", lambda e: e.tensor_copy(out=vs_carry[:], in_=vs_pad[:, 3, :]),
                      reads=["vs_pad.3"], writes=["vs_carry"])
                yield

        def phase3(s):
            for j in range(8):
                if j % 2 == 0:
                    bga, bgb, bys, byg = 2, 3, 0, 7
                else:
                    bga, bgb, bys, byg = 4, 5, 1, 6
                kga, kgb, kys, kyg = pskeys(bga), pskeys(bgb), pskeys(bys), pskeys(byg)
                ca = C_GATE + j * 128
                cb = C_GATE + 1024 + j * 128
                for kc in range(8):
                    S.add("pe", lambda e, kc=kc, ca=ca, bga=bga: e.matmul(
                        PS[bga][:, :], lhsT=w_in_sb[:, kc, ca:ca + 128], rhs=xT[:, kc, :],
                        start=(kc == 0), stop=(kc == 7)),
                        reads=[wgrp(ca)] + XT_ALL, writes=kga)
                for kc in range(8):
                    S.add("pe", lambda e, kc=kc, cb=cb, bgb=bgb: e.matmul(
                        PS[bgb][:, :], lhsT=w_in_sb[:, kc, cb:cb + 128], rhs=xT[:, kc, :],
                        start=(kc == 0), stop=(kc == 7)),
                        reads=[wgrp(cb)] + XT_ALL, writes=kgb)
                for kc in range(4):
                    S.add("pe", lambda e, kc=kc, j=j, bys=bys: e.matmul(
                        PS[bys][:, :], lhsT=w_os_sb[:, kc, j * 128:(j + 1) * 128], rhs=os_gT[:, kc, :],
                        start=(kc == 0), stop=(kc == 3)),
                        reads=["w_os", "os_gT"], writes=kys)
                for kc in range(4):
                    S.add("pe", lambda e, kc=kc, j=j, byg=byg: e.matmul(
                        PS[byg][:, :], lhsT=w_og_sb[:, kc, j * 128:(j + 1) * 128], rhs=og_gT[:, kc, :],
                        start=(kc == 0), stop=(kc == 3)),
                        reads=["w_og", "og_gT"], writes=kyg)
                S.add("act", lambda e, j=j, bga=bga: e.activation(
                    out=ga_sb[:], in_=PS[bga][:, :], func=AF.Sigmoid, bias=bgate[:, j:j + 1]),
                    reads=kga + ["bgate"], writes=["ga_sb"])
                S.add("act", lambda e, j=j, bgb=bgb: e.activation(
                    out=gb_sb[:], in_=PS[bgb][:, :], func=AF.Sigmoid, bias=bgate[:, 8 + j:9 + j]),
                    reads=kgb + ["bgate"], writes=["gb_sb"])
                S.add("dve", lambda e, bys=bys: e.tensor_tensor(out=t2, in0=PS[bys][:, :], in1=gb_sb[:], op=ALU.mult),
                      reads=kys + ["gb_sb"], writes=["t_sb"])
                S.add("dve", lambda e, byg=byg: e.tensor_tensor(out=t1, in0=PS[byg][:, :], in1=ga_sb[:], op=ALU.mult),
                      reads=kyg + ["ga_sb"], writes=["u_sb"])
                S.add("pool", lambda e, j=j: e.tensor_tensor(out=mT[:, j, :], in0=t1, in1=t2, op=ALU.add),
                      reads=["u_sb", "t_sb"], writes=["mT"])

        def phase4(s, b):
            gb = s * 4 + b
            slot = gb % 2
            xk = "xr%d" % slot
            X = xr[slot]
            dma("sp", "xrl%d" % slot, X[:], x_d[gb * 128:(gb + 1) * 128, :], writes=[xk])
            yield
            for half in range(2):
                hsl = slice(half * 512, (half + 1) * 512)
                pb = 4 + half
                for kc in range(8):
                    S.add("pe", lambda e, kc=kc, hsl=hsl, pb=pb: e.matmul(
                        PS[pb][:, :], lhsT=mT[:, kc, b * 128:(b + 1) * 128], rhs=w_out_sb[:, kc, hsl],
                        start=(kc == 0), stop=(kc == 7)),
                        reads=["mT", "w_out"], writes=["ps%d" % pb])
                yield
                S.add("dve", lambda e, hsl=hsl, pb=pb: e.scalar_tensor_tensor(
                    out=X[:, hsl], in0=X[:, hsl], scalar=ALPHA, in1=PS[pb][:, :], op0=ALU.mult, op1=ALU.add),
                    reads=["ps%d" % pb, xk], writes=[xk])
                yield
            S.add("act", lambda e: e.activation(out=X[:], in_=X[:], func=AF.Identity, accum_out=st12[:, 0:1]),
                  reads=[xk], writes=[xk, "st1"])
            S.add("act", lambda e: e.activation(out=junk[:], in_=X[:], func=AF.Square, accum_out=st12[:, 1:2]),
                  reads=[xk], writes=["rs", "st2"])
            yield
            S.add("dve", lambda e: e.tensor_scalar(out=ms12[:], in0=st12[:], scalar1=1.0 / D, scalar2=None, op0=ALU.mult),
                  reads=["st1", "st2"], writes=["ms12"])
            S.add("dve", lambda e: e.scalar_tensor_tensor(
                out=nvar[:], in0=ms12[:, 0:1], scalar=ms12[:, 0:1], in1=ms12[:, 1:2], op0=ALU.mult, op1=ALU.subtract),
                reads=["ms12"], writes=["nvar"])
            yield
            S.add("dve", lambda e: e.tensor_scalar(out=vpe[:], in0=nvar[:], scalar1=-1.0, scalar2=LN_EPS,
                                                   op0=ALU.mult, op1=ALU.add),
                  reads=["nvar"], writes=["vpe"])
            S.add("pool", lambda e: e.tensor_tensor(out=rstd[:], in0=vpe[:], in1=mhalf[:], op=ALU.pow),
                  reads=["vpe", "mhalf"], writes=["rstd"])
            yield
            S.add("dve", lambda e: e.scalar_tensor_tensor(
                out=nbias[:], in0=ms12[:, 0:1], scalar=-1.0, in1=rstd[:], op0=ALU.mult, op1=ALU.mult),
                reads=["ms12", "rstd"], writes=["nbias"])
            yield
            S.add("act", lambda e: e.activation(out=X[:], in_=X[:], func=AF.Identity, scale=rstd[:, 0:1], bias=nbias[:, 0:1]),
                  reads=[xk, "rstd", "nbias"], writes=[xk])
            yield
            S.add("pool", lambda e: e.tensor_tensor(out=X[:], in0=X[:], in1=lng[:], op=ALU.mult),
                  reads=[xk, "lng"], writes=[xk])
            yield
            S.add("pool", lambda e: e.tensor_tensor(out=X[:], in0=X[:], in1=lnb[:], op=ALU.add),
                  reads=[xk, "lnb"], writes=[xk])
            yield
            dma("sp", "st%d" % slot, out_d[gb * 128:(gb + 1) * 128, :], X[:], reads=[xk])
            yield

        def staggered(groups):
            state = []
            for grp in groups:
                gens, lag = grp[0], grp[1]
                init = list(grp[2]) if len(grp) > 2 else [0] * len(gens)
                state.append({"gens": list(gens), "lag": lag, "steps": init, "done": [False] * len(gens)})
            while True:
                progressed = False
                for st in state:
                    for k, g in enumerate(st["gens"]):
                        if st["done"][k]:
                            continue
                        if k > 0 and not st["done"][k - 1] and st["steps"][k - 1] < st["lag"]:
                            continue
                        try:
                            next(g)
                            st["steps"][k] += 1
                        except StopIteration:
                            st["done"][k] = True
                        progressed = True
                if not progressed:
                    break

        def timed(items):
            st = [[g, f, n0, False] for g, f, n0 in items]
            while True:
                best = None
                for k, (g, f, n, done) in enumerate(st):
                    if done:
                        continue
                    t = f(n + 1)
                    if best is None or t < best[0]:
                        best = (t, k)
                if best is None:
                    break
                k = best[1]
                try:
                    next(st[k][0])
                    st[k][2] += 1
                except StopIteration:
                    st[k][3] = True

        def delayed_prefix(gen, delay, nsteps):
            for _ in range(delay):
                yield
            for _ in range(nsteps):
                try:
                    next(gen)
                except StopIteration:
                    return
                yield

        HOIST = int(os.environ.get("KERNEL_HOIST", "8"))
        GL_LAG = int(os.environ.get("KERNEL_GL_LAG", "7"))
        SW_LAG = int(os.environ.get("KERNEL_SW_LAG", "10"))
        P4_LAG = int(os.environ.get("KERNEL_P4_LAG", "7"))
        FRONT_EARLY = int(os.environ.get("KERNEL_FRONT_EARLY", "1"))
        SCHED = int(os.environ.get("KERNEL_SCHED", "2"))
        SP = float(os.environ.get("KERNEL_SP", "14"))
        SF0 = float(os.environ.get("KERNEL_SF0", "6"))
        SFS = float(os.environ.get("KERNEL_SFS", "1.2"))
        SB0 = float(os.environ.get("KERNEL_SB0", "1.5"))
        SBS = float(os.environ.get("KERNEL_SBS", "1.0"))
        SSS = float(os.environ.get("KERNEL_SSS", "1.0"))
        prev = None
        for idx, s in enumerate(sb_list):
            nxt = sb_list[idx + 1] if idx + 1 < len(sb_list) else None
            phase0(s, preloaded=(0, 1, 2, 3), nxt=nxt)
            g_gla = [gla(s, b) for b in range(4)]
            g_swa = [swa(s, b) for b in range(4)]
            p4s = [phase4(prev, b) for b in range(4)] if prev is not None else []
            groups = [([phase1(s)], 0)]
            if p4s:
                groups.append((p4s, P4_LAG))
            if HOIST > 0:
                groups.append(([delayed_prefix(g_gla[0], 11, HOIST)], 0))
            staggered(groups)
            if SCHED == 0:
                def t_gla(b, i):
                    return b * GL_LAG + i - (FRONT_EARLY if (i <= 8 and b > 0) else 0) + 0.5
                def t_swa(b, i):
                    return b * SW_LAG + i + 0.25
            elif SCHED == 2:
                FRONT_T = [3.2, 3.3, 3.4, 8.2, 8.3, 8.4, 11.8, 11.9]
                BACK_T = [1.5, 2.5, 7.5, 7.6, 8.5, 8.6, 9.5, 11.5, 12.5, 13.5, 14.5, 15.5]
                def t_gla(b, i):
                    if i <= 8:
                        return 14.0 * (b - 1) + FRONT_T[i - 1]
                    return 14.0 * b + (BACK_T[i - 9] if i <= 20 else BACK_T[-1] + (i - 20))
                def t_swa(b, i):
                    return 14.0 * b + i
            else:
                def t_gla(b, i):
                    if i <= 8:
                        return SP * (b - 1) + SF0 + (i - 1) * SFS
                    return SP * b + SB0 + (i - 9) * SBS
                def t_swa(b, i):
                    return SP * b + i * SSS
            timed([(g_gla[b], (lambda i, b=b: t_gla(b, i)), (HOIST if b == 0 else 0)) for b in range(4)] +
                  [(g_swa[b], (lambda i, b=b: t_swa(b, i)), 0) for b in range(4)])
            phase3(s)
            prev = s
        staggered([([phase4(prev, b) for b in range(4)], P4_LAG)])

        S.finalize()
        sems = {}
        for key in S.sem_keys:
            sems[key] = es.enter_context(nc.semaphore("s_%s_%s" % key))
        final_waits = [k for k in S.sem_keys if k[0] == "dma" and k[1].startswith("st")]
        with nc.Block() as block:
            S.emit(nc, block, sems, final_waits)
    return nc


def _consts():
    f = np.float32
    ident = np.eye(128, dtype=f)
    m = np.arange(128)
    tri = (m[:, None] <= m[None, :]).astype(f)
    mask = np.tile(tri, (1, 4))
    mbm = np.zeros((128, 4, 4, 128), dtype=f)
    s_ = m[:, None]
    q_ = m[None, :]
    NEG = -30000.0
    for kv in range(2):
        for g in range(4):
            slope = 2.0 ** (-(kv * 4 + g + 1))
            dist_prev = (q_ + 128 - s_).astype(f)
            dist_cur = (q_ - s_).astype(f)
            mbm[:, kv * 2 + 0, g, :] = np.where(s_ > q_, -slope * dist_prev, NEG)
            mbm[:, kv * 2 + 1, g, :] = np.where(s_ <= q_, -slope * dist_cur, NEG)
    E = np.zeros((128, 2, 128), dtype=f)
    E[:, 0, 0:64] = 1.0
    E[:, 1, 64:128] = 1.0
    sel = np.zeros((2, 128), dtype=f)
    sel[0, 0:64] = 1.0
    sel[1, 64:128] = 1.0
    gk = np.zeros((16, 512), dtype=f)
    gk[0, :] = 1.0
    return dict(c_ident=ident, c_mask=np.ascontiguousarray(mask),
                c_mb=np.ascontiguousarray(mbm.reshape(128, 2048)),
                c_E=np.ascontiguousarray(E.reshape(128, 256)), c_sel=sel, c_gk=gk)


def _pair_perm():
    idx = []
    for j in range(4):
        idx.extend(range(j * 64, j * 64 + 64))
        idx.extend(range((4 + j) * 64, (4 + j) * 64 + 64))
    return np.array(idx)


_NC_CACHE = {}


def kernel(x, w_in, w_gk2, b_gk, gla_norm_g, w_o_gla, sinks, w_o_swa, b_gate, w_out, ln_g, ln_b):
    f = np.float32
    nsb = int(os.environ.get("KERNEL_NSB", "16"))
    sb_list = list(range(nsb))
    x = np.asarray(x, dtype=f)
    perm = _pair_perm()
    w_in_p = np.array(w_in, dtype=f, copy=True)
    w_in_p[:, C_QS:C_QS + 512] = np.asarray(w_in)[:, C_QS + perm]
    w_in_p[:, C_ZS:C_ZS + 512] = np.asarray(w_in)[:, C_ZS + perm]
    w_os_p = np.ascontiguousarray(np.asarray(w_o_swa, dtype=f)[perm, :])
    wgk_aug = np.zeros((32, 256), dtype=f)
    wgk_aug[0:16] = np.asarray(w_gk2, dtype=f)
    wgk_aug[16] = np.asarray(b_gk, dtype=f)
    shared = dict(
        w_in=np.ascontiguousarray(w_in_p), w_o_gla=np.ascontiguousarray(np.asarray(w_o_gla, dtype=f)),
        w_o_swa=w_os_p, w_out=np.ascontiguousarray(np.asarray(w_out, dtype=f)), wgk_aug=wgk_aug,
        gnorm=np.ascontiguousarray(np.asarray(gla_norm_g, dtype=f).reshape(128, 1)),
        sinks_rep=np.ascontiguousarray(np.repeat(np.asarray(sinks, dtype=f).reshape(2, 4), 128, axis=1)),
        bgate=np.ascontiguousarray(np.asarray(b_gate, dtype=f).reshape(16, 128).T),
        lng_bc=np.ascontiguousarray(np.broadcast_to(np.asarray(ln_g, dtype=f)[None, :], (128, D))),
        lnb_bc=np.ascontiguousarray(np.broadcast_to(np.asarray(ln_b, dtype=f)[None, :], (128, D))),
    )
    shared.update(_consts())
    key = tuple(sb_list)
    if key not in _NC_CACHE:
        _NC_CACHE[key] = build_nc(sb_list)
    nc = _NC_CACHE[key]
    xs = x.reshape(N_CORES, NTOK, D)
    in_maps = []
    for c in range(N_CORES):
        m = dict(shared)
        m["x"] = np.ascontiguousarray(xs[c])
        in_maps.append(m)
    res = run_bass_kernel_spmd(nc, in_maps, core_ids=list(range(N_CORES)))
    outs = [np.asarray(r["out"], dtype=f).reshape(SEQ_PER_CORE, SEQ, D) for r in res.results]
    return np.concatenate(outs, axis=0).astype(f)
```
